# Optimizing a Trainium2 kernel written in Bass

```python
import math
import jax, jax.numpy as jnp
from jax import lax
import numpy as np

D_MODEL = 1024
BATCH = 8
SEQ = 4096
DEPTH = 2

N_A = DEPTH // 2
N_B = DEPTH - N_A

MLA_HEADS = 8
MLA_Q_LORA = 256
MLA_KV_LORA = 128
MLA_NOPE = 128
MLA_ROPE = 64
MLA_V = 128
MLA_Q_BLOCK = 128

MOBA_HEADS = 8
MOBA_HEAD_DIM = D_MODEL // MOBA_HEADS
MOBA_BLOCK = 256
MOBA_TOPK = 3
MOBA_Q_CHUNK = 16

N_EXPERTS = 32
TOP_K = 4
D_FF = D_MODEL
SWIGLU_LIMIT = 7.0
SWIGLU_ALPHA = 1.702

ROPE_THETA = 10000.0
NORM_EPS = 1e-6
NEG_INF = -1e30

kernel_name = 'hybrid_mla_moba_moe_adaln'


def rms_norm(x, g):
    xf = x.astype(jnp.float32)
    y = xf * lax.rsqrt(jnp.mean(xf * xf, axis=-1, keepdims=True) + NORM_EPS)
    return (y * g.astype(jnp.float32)).astype(x.dtype)


def modulate(h, shift, scale):
    return h * (1.0 + scale[:, None, :]) + shift[:, None, :]


def rope(x, positions):
    d = x.shape[-1]
    half = d // 2
    inv_freq = ROPE_THETA ** (-jnp.arange(half, dtype=jnp.float32) * (2.0 / d))
    ang = positions.astype(jnp.float32)[:, :, None] * inv_freq
    cos = jnp.cos(ang)[:, :, None, :]
    sin = jnp.sin(ang)[:, :, None, :]
    xf = x.astype(jnp.float32)
    x1, x2 = xf[..., :half], xf[..., half:]
    return jnp.concatenate([x1 * cos - x2 * sin, x2 * cos + x1 * sin], axis=-1).astype(x.dtype)


def causal_dense_attention(q, k, v, scale):
    B, S, H, dq = q.shape
    dv = v.shape[-1]
    nq = S // MLA_Q_BLOCK
    qb = q.reshape(B, nq, MLA_Q_BLOCK, H, dq).transpose(1, 0, 2, 3, 4)
    key_pos = jnp.arange(S)

    def one_block(args):
        i, q_blk = args
        s = jnp.einsum('bqhd,bkhd->bhqk', q_blk, k, preferred_element_type=jnp.float32) * scale
        q_pos = i * MLA_Q_BLOCK + jnp.arange(MLA_Q_BLOCK)
        s = jnp.where(key_pos[None, :] <= q_pos[:, None], s, NEG_INF)
        p = jax.nn.softmax(s, axis=-1).astype(v.dtype)
        return jnp.einsum('bhqk,bkhd->bqhd', p, v)

    out = lax.map(one_block, (jnp.arange(nq), qb))
    return out.transpose(1, 0, 2, 3, 4).reshape(B, S, H, dv)


def mla_attention(h, positions, w_in, q_norm_g, w_uq, kv_norm_g, w_ukv, w_o):
    B, S, _ = h.shape
    H = MLA_HEADS
    proj = h @ w_in
    c_q, c_kv, k_rope = jnp.split(proj, [MLA_Q_LORA, MLA_Q_LORA + MLA_KV_LORA], axis=-1)
    q = (rms_norm(c_q, q_norm_g) @ w_uq).reshape(B, S, H, MLA_NOPE + MLA_ROPE)
    q = jnp.concatenate([q[..., :MLA_NOPE], rope(q[..., MLA_NOPE:], positions)], axis=-1)
    kv = (rms_norm(c_kv, kv_norm_g) @ w_ukv).reshape(B, S, H, MLA_NOPE + MLA_V)
    k_nope, v = kv[..., :MLA_NOPE], kv[..., MLA_NOPE:]
    k_rope = rope(k_rope[:, :, None, :], positions)
    k = jnp.concatenate([k_nope, jnp.broadcast_to(k_rope, (B, S, H, MLA_ROPE))], axis=-1)
    out = causal_dense_attention(q, k, v, (MLA_NOPE + MLA_ROPE) ** -0.5)
    return out.reshape(B, S, H * MLA_V) @ w_o


def moba_shared_kv(h_kv, positions, w_kv):
    B, S, _ = h_kv.shape
    H, HD = MOBA_HEADS, MOBA_HEAD_DIM
    kv = (h_kv @ w_kv).reshape(B, S, 2, H, HD)
    k = rope(kv[:, :, 0], positions)
    v = kv[:, :, 1]
    nb = -(-S // MOBA_BLOCK)
    pad = nb * MOBA_BLOCK - S
    k = jnp.pad(k, ((0, 0), (0, pad), (0, 0), (0, 0)))
    v = jnp.pad(v, ((0, 0), (0, pad), (0, 0), (0, 0)))
    k_blocks = k.reshape(B, nb, MOBA_BLOCK, H, HD).transpose(0, 3, 1, 2, 4)
    v_blocks = v.reshape(B, nb, MOBA_BLOCK, H, HD).transpose(0, 3, 1, 2, 4)
    k_mean = jnp.mean(k_blocks.astype(jnp.float32), axis=3).astype(k.dtype)
    return k_blocks, v_blocks, k_mean


def moba_attention(h, positions, w_q, w_o, k_blocks, v_blocks, k_mean):
    B, S, _ = h.shape
    H, HD, BLK, QC = MOBA_HEADS, MOBA_HEAD_DIM, MOBA_BLOCK, MOBA_Q_CHUNK
    nb = k_blocks.shape[2]
    topk = min(MOBA_TOPK, nb)
    scale = HD ** -0.5
    q = rope((h @ w_q).reshape(B, S, H, HD), positions)
    nc = S // QC
    qc = q.reshape(B, nc, QC, H, HD).transpose(1, 0, 3, 2, 4)
    b_idx = jnp.arange(B)[:, None, None, None]
    h_idx = jnp.arange(H)[None, :, None, None]

    def one_chunk(args):
        ci, q_c = args
        start = ci * QC
        own = start // BLK
        gate = jnp.einsum('bhqd,bhnd->bhqn', q_c, k_mean, preferred_element_type=jnp.float32)
        gate = jnp.where(jnp.arange(nb) < own, gate, -jnp.inf)
        _, sel = lax.top_k(gate, topk)
        sel_valid = jnp.arange(topk) < own
        k_sel = k_blocks[b_idx, h_idx, sel]
        v_sel = v_blocks[b_idx, h_idx, sel]
        s_sel = jnp.einsum('bhqd,bhqnkd->bhqnk', q_c, k_sel, preferred_element_type=jnp.float32) * scale
        s_sel = jnp.where(sel_valid[:, None], s_sel, NEG_INF).reshape(B, H, QC, topk * BLK)
        k_own = lax.dynamic_index_in_dim(k_blocks, own, axis=2, keepdims=False)
        v_own = lax.dynamic_index_in_dim(v_blocks, own, axis=2, keepdims=False)
        s_own = jnp.einsum('bhqd,bhkd->bhqk', q_c, k_own, preferred_element_type=jnp.float32) * scale
        q_pos = start + jnp.arange(QC)
        k_pos = own * BLK + jnp.arange(BLK)
        s_own = jnp.where(k_pos[None, :] <= q_pos[:, None], s_own, NEG_INF)
        p = jax.nn.softmax(jnp.concatenate([s_sel, s_own], axis=-1), axis=-1).astype(v_blocks.dtype)
        p_sel = p[..., :topk * BLK].reshape(B, H, QC, topk, BLK)
        p_own = p[..., topk * BLK:]
        return (jnp.einsum('bhqnk,bhqnkd->bhqd', p_sel, v_sel)
                + jnp.einsum('bhqk,bhkd->bhqd', p_own, v_own))

    out = lax.map(one_chunk, (jnp.arange(nc), qc))
    out = out.transpose(1, 0, 3, 2, 4).reshape(B, S, H * HD)
    return out @ w_o


def moe_ffn(h, router_w, router_b, w_gate, b_gate, w_up, b_up, w_down, b_down):
    B, S, D = h.shape
    t = h.reshape(B * S, D)
    logits = (t @ router_w + router_b).astype(jnp.float32)
    top_vals, top_idx = lax.top_k(logits, TOP_K)
    top_w = jax.nn.softmax(top_vals, axis=-1)
    gates = jnp.sum(jax.nn.one_hot(top_idx, N_EXPERTS, dtype=jnp.float32) * top_w[..., None], axis=-2)
    out = jnp.zeros((B * S, D), jnp.float32)
    for e in range(N_EXPERTS):
        g = jnp.minimum(t @ w_gate[e] + b_gate[e], SWIGLU_LIMIT)
        u = jnp.clip(t @ w_up[e] + b_up[e], -SWIGLU_LIMIT, SWIGLU_LIMIT)
        a = g * jax.nn.sigmoid(SWIGLU_ALPHA * g) * (u + 1.0)
        out = out + gates[:, e:e + 1] * (a @ w_down[e] + b_down[e])
    return out.astype(h.dtype).reshape(B, S, D)


def setup_inputs(seed: int = 0) -> dict:
    key = jax.random.key(seed)
    keys = iter(jax.random.split(key, 40))

    def nrm(shape, scale):
        return jax.random.normal(next(keys), shape, jnp.float32) * scale

    def gain(shape):
        return 1.0 + nrm(shape, 0.05)

    D = D_MODEL
    HM = MLA_HEADS
    HB = MOBA_HEADS * MOBA_HEAD_DIM
    x = nrm((BATCH, SEQ, D), 1.0)
    c = nrm((BATCH, D), 1.0)
    positions = jnp.tile(jnp.arange(SEQ, dtype=jnp.int32)[None, :], (BATCH, 1))
    return {
        'x': x,
        'c': c,
        'positions': positions,
        'ada_w': nrm((DEPTH, D, 6 * D), 0.5 * D ** -0.5),
        'ada_b': nrm((DEPTH, 6 * D), 0.02),
        'norm_attn_g': gain((DEPTH, D)),
        'norm_ffn_g': gain((DEPTH, D)),
        'mla_w_in': nrm((N_A, D, MLA_Q_LORA + MLA_KV_LORA + MLA_ROPE), D ** -0.5),
        'mla_q_norm_g': gain((N_A, MLA_Q_LORA)),
        'mla_w_uq': nrm((N_A, MLA_Q_LORA, HM * (MLA_NOPE + MLA_ROPE)), MLA_Q_LORA ** -0.5),
        'mla_kv_norm_g': gain((N_A, MLA_KV_LORA)),
        'mla_w_ukv': nrm((N_A, MLA_KV_LORA, HM * (MLA_NOPE + MLA_V)), MLA_KV_LORA ** -0.5),
        'mla_w_o': nrm((N_A, HM * MLA_V, D), (HM * MLA_V) ** -0.5),
        'kv_ada_w': nrm((D, 2 * D), 0.5 * D ** -0.5),
        'kv_ada_b': nrm((2 * D,), 0.02),
        'kv_norm_g': gain((D,)),
        'moba_w_kv': nrm((D, 2 * HB), D ** -0.5),
        'moba_w_q': nrm((N_B, D, HB), D ** -0.5),
        'moba_w_o': nrm((N_B, HB, D), HB ** -0.5),
        'router_w': nrm((DEPTH, D, N_EXPERTS), D ** -0.5),
        'router_b': nrm((DEPTH, N_EXPERTS), 0.01),
        'w_gate': nrm((DEPTH, N_EXPERTS, D, D_FF), D ** -0.5),
        'b_gate': nrm((DEPTH, N_EXPERTS, D_FF), 0.02),
        'w_up': nrm((DEPTH, N_EXPERTS, D, D_FF), D ** -0.5),
        'b_up': nrm((DEPTH, N_EXPERTS, D_FF), 0.02),
        'w_down': nrm((DEPTH, N_EXPERTS, D_FF, D), D_FF ** -0.5),
        'b_down': nrm((DEPTH, N_EXPERTS, D), 0.02),
        'final_ada_w': nrm((D, 2 * D), 0.5 * D ** -0.5),
        'final_ada_b': nrm((2 * D,), 0.02),
        'final_norm_g': gain((D,)),
    }


def reference(x, c, positions, ada_w, ada_b, norm_attn_g, norm_ffn_g,
              mla_w_in, mla_q_norm_g, mla_w_uq, mla_kv_norm_g, mla_w_ukv, mla_w_o,
              kv_ada_w, kv_ada_b, kv_norm_g, moba_w_kv, moba_w_q, moba_w_o,
              router_w, router_b, w_gate, b_gate, w_up, b_up, w_down, b_down,
              final_ada_w, final_ada_b, final_norm_g):
    c_act = jax.nn.silu(c)
    shared = None
    for layer in range(DEPTH):
        mod = c_act @ ada_w[layer] + ada_b[layer]
        sh_a, sc_a, g_a, sh_f, sc_f, g_f = jnp.split(mod, 6, axis=-1)
        h = modulate(rms_norm(x, norm_attn_g[layer]), sh_a, sc_a)
        if layer < N_A:
            a = mla_attention(h, positions, mla_w_in[layer], mla_q_norm_g[layer], mla_w_uq[layer],
                              mla_kv_norm_g[layer], mla_w_ukv[layer], mla_w_o[layer])
        else:
            if shared is None:
                kv_shift, kv_scale = jnp.split(c_act @ kv_ada_w + kv_ada_b, 2, axis=-1)
                h_kv = modulate(rms_norm(x, kv_norm_g), kv_shift, kv_scale)
                shared = moba_shared_kv(h_kv, positions, moba_w_kv)
            j = layer - N_A
            a = moba_attention(h, positions, moba_w_q[j], moba_w_o[j], shared[0], shared[1], shared[2])
        x = x + g_a[:, None, :] * a
        h = modulate(rms_norm(x, norm_ffn_g[layer]), sh_f, sc_f)
        f = moe_ffn(h, router_w[layer], router_b[layer], w_gate[layer], b_gate[layer],
                    w_up[layer], b_up[layer], w_down[layer], b_down[layer])
        x = x + g_f[:, None, :] * f
    f_shift, f_scale = jnp.split(c_act @ final_ada_w + final_ada_b, 2, axis=-1)
    return modulate(rms_norm(x, final_norm_g), f_shift, f_scale)
```

```python
import contextlib
import numpy as np
import concourse.bass as bass
import concourse.mybir as mybir
from concourse.bass_utils import run_bass_kernel_spmd

F32 = mybir.dt.float32
BF16 = mybir.dt.bfloat16
I32 = mybir.dt.int32
AF = mybir.ActivationFunctionType
ALU = mybir.AluOpType
AX = mybir.AxisListType

S = 4096
D = 1024
NT = S // 512
PI = float(np.pi)
TWO_PI = float(2 * np.pi)
NEG = -30000.0


class Sem:
    def __init__(self, h, idx):
        self.h = h
        self.idx = idx
        self.count = 0


class Buf:
    __slots__ = ("name", "last_w", "readers", "dsem")

    def __init__(self, name=""):
        self.name = name
        self.last_w = None
        self.readers = {}
        self.dsem = {}


class Eng:
    def __init__(self, name, eng, sem):
        self.name = name
        self.eng = eng
        self.sem = sem
        self.seen = {}


class FW:
    def __init__(self, nc, stack):
        self.nc = nc
        self.stack = stack
        self.nsem = 0
        self.E = {}
        for name, attr in (("pe", "tensor"), ("act", "scalar"), ("dve", "vector"),
                           ("pool", "gpsimd"), ("sp", "sync")):
            self.E[name] = Eng(name, getattr(nc, attr), self.new_sem("e_" + name))
        self.dma_sems = []
        self.free_dsems = {"hw": [], "sw": []}
        self.nwaits = 0
        self.ninst = 0
        self.uid = 0

    def new_sem(self, name):
        h = self.stack.enter_context(self.nc.semaphore(f"{name}_{self.nsem}"))
        s = Sem(h, self.nsem)
        self.nsem += 1
        return s

    def buf(self, name=""):
        return Buf(name)

    def bufs(self, n, name=""):
        return [Buf(f"{name}{i}") for i in range(n)]

    def _wait(self, E, deps):
        for S_, v in deps.values():
            if E.name == "pe" and S_ is E.sem:
                continue
            if E.seen.get(S_.idx, 0) < v:
                E.eng.wait_ge(S_.h, v)
                E.seen[S_.idx] = v
                self.nwaits += 1

    @staticmethod
    def _add(deps, d):
        if d is None:
            return
        S_, v = d
        cur = deps.get(S_.idx)
        if cur is None or cur[1] < v:
            deps[S_.idx] = (S_, v)

    def _deps(self, reads, writes):
        deps = {}
        for b in reads:
            self._add(deps, b.last_w)
        for b in writes:
            self._add(deps, b.last_w)
            for d in b.readers.values():
                self._add(deps, d)
        return deps

    def op(self, ename, fn, reads=(), writes=(), inc=True):
        E = self.E[ename]
        self._wait(E, self._deps(reads, writes))
        inst = fn(E.eng)
        self.ninst += 1
        if inc:
            E.sem.count += 1
            inst.then_inc(E.sem.h, 1)
            done = (E.sem, E.sem.count)
        else:
            done = (E.sem, E.sem.count + 1)
        for b in writes:
            b.last_w = done
            b.readers = {}
        for b in reads:
            b.readers[E.sem.idx] = done
        return inst

    def _dsem(self, qname, owner):
        kind = "sw" if qname == "pool" else "hw"
        S_ = owner.dsem.get(kind)
        if S_ is None:
            if self.free_dsems[kind]:
                S_ = self.free_dsems[kind].pop()
            else:
                S_ = self.new_sem("d" + kind)
                self.dma_sems.append(S_)
            owner.dsem[kind] = S_
        return S_

    def dma(self, qname, out, in_, reads=(), writes=(), sem_owner=None, **kw):
        return self.dma_custom(qname, lambda eng: eng.dma_start(out=out, in_=in_, **kw), reads, writes, sem_owner)

    def dma_custom(self, qname, fn, reads=(), writes=(), sem_owner=None):
        E = self.E[qname]
        owner = sem_owner or (writes[0] if writes else reads[0])
        S_ = self._dsem(qname, owner)
        self._wait(E, self._deps(reads, writes))
        inst = fn(E.eng)
        self.ninst += 1
        S_.count += 16
        inst.then_inc(S_.h, 16)
        done = (S_, S_.count)
        for b in writes:
            b.last_w = done
            b.readers = {}
        for b in reads:
            b.readers[S_.idx] = done
        return inst

    def barrier(self, release=()):
        sems = [e.sem for e in self.E.values()] + list(self.dma_sems)
        for E in self.E.values():
            deps = {}
            for S_ in sems:
                if S_.count > 0:
                    deps[S_.idx] = (S_, S_.count)
            self._wait(E, deps)
        for b in release:
            for kind, S_ in b.dsem.items():
                self.free_dsems[kind].append(S_)
            b.dsem = {}

    def name(self, base):
        self.uid += 1
        return f"{base}_{self.uid}"


class K:
    pass


def sbt(k, st, name, shape, dt):
    return st.enter_context(k.nc.sbuf_tensor(k.fw.name(name), shape, dt))


V_MOD = (0, 48)
V_KVMOD = 96
V_FINMOD = 112
V_GAIN = 128
V_QG = 176
V_KVG2 = 178
V_GSA = (180, 188)
V_GSF = (196, 204)
V_GSKV = 212
V_GSFIN = 220
NV = 256
R_ADAB = (0, 6144)
R_KVB = 12288
R_FINB = 14336
R_GAIN = 16384
R_QG = 22528
R_KVG2 = 22784
NROWS = 22912


def phase0(k):
    nc, fw = k.nc, k.fw
    with contextlib.ExitStack() as st:
        rows = sbt(k, st, "rows", [1, NROWS], F32)
        rows_b = fw.buf("rows")
        wblk = [sbt(k, st, "adaw", [128, 8, 1024], F32) for _ in range(2)]
        wblk_b = fw.bufs(2, "adaw")
        cT = sbt(k, st, "cT", [128, 8], F32)
        cA = sbt(k, st, "cA", [128, 8], F32)
        sg = sbt(k, st, "csg", [128, 8], F32)
        cT_b, cA_b, sg_b = fw.buf("cT"), fw.buf("cA"), fw.buf("csg")
        d = k.d
        loads = [(R_ADAB[0], d["ada_b"][0:1, :], 6144), (R_ADAB[1], d["ada_b"][1:2, :], 6144),
                 (R_KVB, d["kv_ada_b"], 2048), (R_FINB, d["final_ada_b"], 2048),
                 (R_GAIN + 0, d["norm_attn_g"][0:1, :], 1024), (R_GAIN + 1024, d["norm_attn_g"][1:2, :], 1024),
                 (R_GAIN + 2048, d["norm_ffn_g"][0:1, :], 1024), (R_GAIN + 3072, d["norm_ffn_g"][1:2, :], 1024),
                 (R_GAIN + 4096, d["kv_norm_g"], 1024), (R_GAIN + 5120, d["final_norm_g"], 1024),
                 (R_QG, d["mla_q_norm_g"], 256), (R_KVG2, d["mla_kv_norm_g"], 128)]
        for off, ap, n in loads:
            fw.dma("sp", out=rows[0:1, off:off + n], in_=ap, writes=[rows_b])
        fw.dma("sp", out=cT[:], in_=d["cT"], writes=[cT_b])
        fw.op("act", lambda e: e.activation(out=sg[:], in_=cT[:], func=AF.Sigmoid), reads=[cT_b], writes=[sg_b])
        fw.op("dve", lambda e: e.tensor_tensor(out=cA[:], in0=cT[:], in1=sg[:], op=ALU.mult),
              reads=[cT_b, sg_b], writes=[cA_b])
        ps, psb = k.ps[0], k.psb[0]
        one11 = k.ones_row[0:1, 0:1]
        mats = [(d["ada_w"][0], 6, V_MOD[0], R_ADAB[0]), (d["ada_w"][1], 6, V_MOD[1], R_ADAB[1]),
                (d["kv_ada_w"], 2, V_KVMOD, R_KVB), (d["final_ada_w"], 2, V_FINMOD, R_FINB)]
        blocks = []
        for w_ap, nb, vcol, roff in mats:
            for cb in range(nb):
                blocks.append((w_ap, cb, vcol + cb * 8, roff + cb * 1024))

        def load_blk(i):
            w_ap, cb, _, _ = blocks[i]
            fw.dma("sp", out=wblk[i % 2][:],
                   in_=w_ap[:, cb * 1024:(cb + 1) * 1024].rearrange("(kc p) f -> p kc f", p=128),
                   writes=[wblk_b[i % 2]])
        load_blk(0)
        for i, (w_ap, cb, vcol, roff) in enumerate(blocks):
            if i + 1 < len(blocks):
                load_blk(i + 1)
            W, Wb = wblk[i % 2], wblk_b[i % 2]
            for j in range(8):
                col = vcol + j
                for kc in range(8):
                    fw.op("pe", lambda e: e.matmul(ps[:, col:col + 1], lhsT=W[:, kc, j * 128:(j + 1) * 128],
                                                   rhs=cA[:, kc:kc + 1], start=(kc == 0), stop=False),
                          reads=[Wb, cA_b], writes=[psb], inc=False)
                fw.op("pe", lambda e: e.matmul(ps[:, col:col + 1], lhsT=rows[0:1, roff + j * 128: roff + (j + 1) * 128],
                                               rhs=one11, start=False, stop=True),
                      reads=[rows_b, k.const_b], writes=[psb])
        for i in range(6 * 8 + 3):
            col = V_GAIN + i
            roff = R_GAIN + i * 128
            fw.op("pe", lambda e: e.matmul(ps[:, col:col + 1], lhsT=rows[0:1, roff:roff + 128], rhs=one11,
                                           start=True, stop=True),
                  reads=[rows_b, k.const_b], writes=[psb])
        vec, vb = k.vec, k.vec_b
        fw.op("dve", lambda e: e.tensor_copy(out=vec[:, 0:180], in_=ps[:, 0:180]), reads=[psb], writes=[vb])
        for (dst, gcol, sccol) in ((V_GSA[0], V_GAIN + 0, V_MOD[0] + 8), (V_GSA[1], V_GAIN + 8, V_MOD[1] + 8),
                                   (V_GSF[0], V_GAIN + 16, V_MOD[0] + 32), (V_GSF[1], V_GAIN + 24, V_MOD[1] + 32),
                                   (V_GSKV, V_GAIN + 32, V_KVMOD + 8), (V_GSFIN, V_GAIN + 40, V_FINMOD + 8)):
            fw.op("dve", lambda e: e.scalar_tensor_tensor(out=vec[:, dst:dst + 8], in0=vec[:, sccol:sccol + 8], scalar=1.0,
                                                          in1=vec[:, gcol:gcol + 8], op0=ALU.add, op1=ALU.mult),
                  reads=[vb], writes=[vb])
        fw.barrier(release=[rows_b, cT_b] + wblk_b)


def build_trig(k, st, cosT, sinT, tb, npart, invf_col, sign_col):
    nc, fw = k.nc, k.fw
    posi = sbt(k, st, "posi", [1, S], I32)
    posf = sbt(k, st, "posf", [1, S], F32)
    pb = fw.buf("pos")
    fw.dma("sp", out=posi[:], in_=k.d["pos"], writes=[pb])
    fw.op("dve", lambda e: e.tensor_copy(out=posf[:], in_=posi[:]), reads=[pb], writes=[pb])
    ang = sbt(k, st, "ang", [128, 512], F32)
    r = sbt(k, st, "rr", [128, 512], F32)
    ki = sbt(k, st, "ki", [128, 512], I32)
    kf = sbt(k, st, "kf", [128, 512], F32)
    m = sbt(k, st, "mm", [128, 512], F32)
    wb = fw.buf("trigw")
    P = npart
    for t in range(NT):
        ts_ = slice(t * 512, (t + 1) * 512)
        ps, psb = k.ps[t % 2], k.psb[t % 2]
        fw.op("pe", lambda e: e.matmul(ps[0:P, :], lhsT=k.ones_row[0:1, 0:P], rhs=posf[0:1, ts_], start=True, stop=True),
              reads=[pb, k.const_b], writes=[psb])
        for which, dst in ((0, sinT), (1, cosT)):
            if which == 0:
                fw.op("dve", lambda e: e.tensor_scalar(out=ang[0:P, :], in0=ps[0:P, :], scalar1=k.cvec[0:P, invf_col:invf_col + 1],
                                                       scalar2=None, op0=ALU.mult), reads=[psb, k.const_b], writes=[wb])
            else:
                fw.op("dve", lambda e: e.tensor_scalar(out=ang[0:P, :], in0=ps[0:P, :], scalar1=k.cvec[0:P, invf_col:invf_col + 1],
                                                       scalar2=PI / 2, op0=ALU.mult, op1=ALU.add), reads=[psb, k.const_b], writes=[wb])
            fw.op("dve", lambda e: e.tensor_scalar(out=ki[0:P, :], in0=ang[0:P, :], scalar1=1.0 / TWO_PI, scalar2=None, op0=ALU.mult),
                  reads=[wb], writes=[wb])
            fw.op("dve", lambda e: e.tensor_copy(out=kf[0:P, :], in_=ki[0:P, :]), reads=[wb], writes=[wb])
            fw.op("dve", lambda e: e.scalar_tensor_tensor(out=r[0:P, :], in0=kf[0:P, :], scalar=-TWO_PI, in1=ang[0:P, :],
                                                          op0=ALU.mult, op1=ALU.add), reads=[wb], writes=[wb])
            fw.op("dve", lambda e: e.tensor_scalar(out=m[0:P, :], in0=r[0:P, :], scalar1=PI, scalar2=None, op0=ALU.is_gt),
                  reads=[wb], writes=[wb])
            fw.op("dve", lambda e: e.scalar_tensor_tensor(out=r[0:P, :], in0=m[0:P, :], scalar=-TWO_PI, in1=r[0:P, :],
                                                          op0=ALU.mult, op1=ALU.add), reads=[wb], writes=[wb])
            fw.op("dve", lambda e: e.tensor_scalar(out=m[0:P, :], in0=r[0:P, :], scalar1=-PI, scalar2=None, op0=ALU.is_lt),
                  reads=[wb], writes=[wb])
            fw.op("dve", lambda e: e.scalar_tensor_tensor(out=r[0:P, :], in0=m[0:P, :], scalar=TWO_PI, in1=r[0:P, :],
                                                          op0=ALU.mult, op1=ALU.add), reads=[wb], writes=[wb])
            fw.op("dve", lambda e: e.tensor_scalar(out=r[0:P, :], in0=r[0:P, :], scalar1=3.14159, scalar2=-3.14159,
                                                   op0=ALU.min, op1=ALU.max), reads=[wb], writes=[wb])
            if which == 0:
                fw.op("act", lambda e: e.activation(out=dst[0:P, ts_], in_=r[0:P, :], func=AF.Sin,
                                                    scale=k.cvec[0:P, sign_col:sign_col + 1]),
                      reads=[wb, k.const_b], writes=[tb])
            else:
                fw.op("act", lambda e: e.activation(out=dst[0:P, ts_], in_=r[0:P, :], func=AF.Sin),
                      reads=[wb], writes=[tb])


class NormRes:
    pass


def norm_alloc(k, st, T):
    R = NormRes()
    R.sq = sbt(k, st, "n_sq", [128, 8, 512], BF16)
    R.rstd = sbt(k, st, "n_rstd", [128, 512], F32)
    R.tmp = [sbt(k, st, "n_tmp", [128, 512], F32) for _ in range(2)]
    R.sqb, R.rstdb = k.fw.buf("n_sq"), k.fw.buf("n_rstd")
    R.tmpb = k.fw.bufs(2, "n_tmp")
    R.i = 0
    return R


def norm_rstd(k, R, xT, xb, ts_, psi):
    fw = k.fw
    ps, psb = k.ps[psi], k.psb[psi]
    fw.op("act", lambda e: e.activation(out=R.sq[:], in_=xT[:, :, ts_], func=AF.Square), reads=[xb], writes=[R.sqb])
    for kc in range(8):
        fw.op("pe", lambda e: e.matmul(ps[:], lhsT=k.ones_b[:], rhs=R.sq[:, kc, :], start=(kc == 0), stop=(kc == 7)),
              reads=[R.sqb, k.const_b], writes=[psb], inc=(kc == 7))
    fw.op("act", lambda e: e.activation(out=R.rstd[:], in_=ps[:], func=AF.Sqrt, scale=1.0 / D, bias=k.eps_col[:]),
          reads=[psb, k.const_b], writes=[R.rstdb])
    fw.op("dve", lambda e: e.reciprocal(out=R.rstd[:], in_=R.rstd[:]), reads=[R.rstdb], writes=[R.rstdb])


def norm_apply(k, R, xT, xb, ts_, gs_col, sh_col, out_fn, out_bufs):
    fw = k.fw
    for kc in range(8):
        i = R.i % 2
        R.i += 1
        fw.op("dve", lambda e: e.scalar_tensor_tensor(out=R.tmp[i][:], in0=xT[:, kc, ts_], scalar=k.vec[:, gs_col + kc:gs_col + kc + 1],
                                                      in1=R.rstd[:], op0=ALU.mult, op1=ALU.mult),
              reads=[xb, R.rstdb, k.vec_b], writes=[R.tmpb[i]])
        fw.op("act", lambda e: e.activation(out=out_fn(kc), in_=R.tmp[i][:], func=AF.Identity,
                                            bias=k.vec[:, sh_col + kc:sh_col + kc + 1]),
              reads=[R.tmpb[i], k.vec_b], writes=[out_bufs[kc] if isinstance(out_bufs, list) else out_bufs])


def load_w_bf16(k, dst, dstb, src_ap):
    k.fw.dma("pool", out=dst, in_=src_ap, writes=[dstb])


def phase_mla(k):
    nc, fw, d = k.nc, k.fw, k.d
    with contextlib.ExitStack() as st:
        cos64 = sbt(k, st, "cos64", [64, S], F32)
        sin64 = sbt(k, st, "sin64", [64, S], F32)
        trig_b = fw.buf("trig64")
        cqn = sbt(k, st, "cqn", [128, 2, S], BF16)
        ckvn = sbt(k, st, "ckvn", [128, S], BF16)
        krT = sbt(k, st, "krT", [64, S], BF16)
        lat_b = [fw.buf(f"lat{t}") for t in range(NT)]
        OT = sbt(k, st, "OT", [128, 8, S], BF16)
        OT_b = [[fw.buf(f"OT{h}_{t}") for t in range(NT)] for h in range(8)]
        with contextlib.ExitStack() as st1:
            build_trig(k, st1, cos64, sin64, trig_b, 64, 0, 2)
            fw.barrier()
        with contextlib.ExitStack() as st1:
            w_in = sbt(k, st1, "w_in", [128, 8, 448], BF16)
            w_inR = sbt(k, st1, "w_inR", [128, 8, 64], BF16)
            wb = fw.buf("w_in")
            win_ap = d["mla_w_in"][0].rearrange("(kc p) f -> p kc f", p=128)
            fw.dma("pool", out=w_in[:], in_=win_ap, writes=[wb])
            fw.dma("pool", out=w_inR[:, :, 0:32], in_=win_ap[:, :, 416:448], writes=[wb])
            fw.dma("pool", out=w_inR[:, :, 32:64], in_=win_ap[:, :, 384:416], writes=[wb])
            xin = [sbt(k, st1, "xin", [128, D], F32) for _ in range(2)]
            xin_b = fw.bufs(2, "xin")
            xTt = sbt(k, st1, "xTt", [128, 8, 512], F32)
            xTt_b = fw.buf("xTt")
            hTt = sbt(k, st1, "hTt", [128, 8, 512], BF16)
            hTt_b = fw.buf("hTt")
            NR = norm_alloc(k, st1, 512)
            sqq = sbt(k, st1, "sqq", [128, 3, 512], BF16)
            sqq_b = fw.buf("sqq")
            rs2 = sbt(k, st1, "rs2", [128, 2, 512], F32)
            rs2_b = fw.buf("rs2")
            t1 = sbt(k, st1, "t1", [64, 512], F32)
            t2 = sbt(k, st1, "t2", [64, 512], F32)
            t_b = fw.buf("t12")
            ps, psb = k.ps, k.psb
            for t in range(NT):
                ts_ = slice(t * 512, (t + 1) * 512)
                for sub in range(4):
                    i = sub % 2
                    r0 = t * 512 + sub * 128
                    fw.dma("sp", out=xin[i][:], in_=d["x"][r0:r0 + 128, :], writes=[xin_b[i]])
                    for half in range(2):
                        pi = 6 + half
                        for q in range(4):
                            kc = half * 4 + q
                            fw.op("pe", lambda e: e.transpose(out=ps[pi][:, q * 128:(q + 1) * 128],
                                                              in_=xin[i][:, kc * 128:(kc + 1) * 128], identity=k.ident_f[:]),
                                  reads=[xin_b[i], k.const_b], writes=[psb[pi]], inc=(q == 3))
                        fw.op("act", lambda e: e.activation(out=xTt[:, half * 4:half * 4 + 4, sub * 128:(sub + 1) * 128],
                                                            in_=ps[pi][:].rearrange("p (q c) -> p q c", q=4), func=AF.Copy),
                              reads=[psb[pi]], writes=[xTt_b])
                fw.dma("sp", out=k.xT_d[:, :, ts_], in_=xTt[:], reads=[xTt_b], writes=[k.xT_db[t]])
                tsl = slice(0, 512)
                norm_rstd(k, NR, xTt, xTt_b, tsl, 0)
                norm_apply(k, NR, xTt, xTt_b, tsl, V_GSA[0], V_MOD[0] + 0, lambda kc: hTt[:, kc, :], hTt_b)
                outs = [(1, slice(0, 128), w_in, 128), (2, slice(128, 256), w_in, 128), (3, slice(256, 384), w_in, 128),
                        (4, slice(384, 448), w_in, 64), (5, slice(0, 64), w_inR, 64)]
                for pi, cs, W, M in outs:
                    for kc in range(8):
                        fw.op("pe", lambda e: e.matmul(ps[pi][0:M, :], lhsT=W[:, kc, cs], rhs=hTt[:, kc, :],
                                                       start=(kc == 0), stop=(kc == 7)),
                              reads=[wb, hTt_b], writes=[psb[pi]], inc=(kc == 7))
                for j, pi in enumerate((1, 2, 3)):
                    fw.op("act", lambda e: e.activation(out=sqq[:, j, :], in_=ps[pi][:], func=AF.Square),
                          reads=[psb[pi]], writes=[sqq_b])
                fw.op("pe", lambda e: e.matmul(ps[0][:], lhsT=k.ones_b[:], rhs=sqq[:, 0, :], start=True, stop=False),
                      reads=[sqq_b, k.const_b], writes=[psb[0]], inc=False)
                fw.op("pe", lambda e: e.matmul(ps[0][:], lhsT=k.ones_b[:], rhs=sqq[:, 1, :], start=False, stop=True),
                      reads=[sqq_b, k.const_b], writes=[psb[0]])
                fw.op("pe", lambda e: e.matmul(ps[6][:], lhsT=k.ones_b[:], rhs=sqq[:, 2, :], start=True, stop=True),
                      reads=[sqq_b, k.const_b], writes=[psb[6]])
                fw.op("act", lambda e: e.activation(out=rs2[:, 0, :], in_=ps[0][:], func=AF.Sqrt, scale=1.0 / 256, bias=k.eps_col[:]),
                      reads=[psb[0], k.const_b], writes=[rs2_b])
                fw.op("act", lambda e: e.activation(out=rs2[:, 1, :], in_=ps[6][:], func=AF.Sqrt, scale=1.0 / 128, bias=k.eps_col[:]),
                      reads=[psb[6], k.const_b], writes=[rs2_b])
                fw.op("dve", lambda e: e.reciprocal(out=rs2[:], in_=rs2[:]), reads=[rs2_b], writes=[rs2_b])
                for j, pi in enumerate((1, 2)):
                    fw.op("dve", lambda e: e.scalar_tensor_tensor(out=cqn[:, j, ts_], in0=ps[pi][:], scalar=k.vec[:, V_QG + j:V_QG + j + 1],
                                                                  in1=rs2[:, 0, :], op0=ALU.mult, op1=ALU.mult),
                          reads=[psb[pi], rs2_b, k.vec_b], writes=[lat_b[t]])
                fw.op("dve", lambda e: e.scalar_tensor_tensor(out=ckvn[:, ts_], in0=ps[3][:], scalar=k.vec[:, V_KVG2:V_KVG2 + 1],
                                                              in1=rs2[:, 1, :], op0=ALU.mult, op1=ALU.mult),
                      reads=[psb[3], rs2_b, k.vec_b], writes=[lat_b[t]])
                fw.op("dve", lambda e: e.tensor_tensor(out=t1[:], in0=ps[4][0:64, :], in1=cos64[:, ts_], op=ALU.mult),
                      reads=[psb[4], trig_b], writes=[t_b])
                fw.op("dve", lambda e: e.tensor_tensor(out=t2[:], in0=ps[5][0:64, :], in1=sin64[:, ts_], op=ALU.mult),
                      reads=[psb[5], trig_b], writes=[t_b])
                fw.op("dve", lambda e: e.tensor_tensor(out=krT[:, ts_], in0=t1[:], in1=t2[:], op=ALU.add),
                      reads=[t_b], writes=[lat_b[t]])
            fw.barrier(release=xin_b + [wb])
        if k.stop == "1a":
            return
        with contextlib.ExitStack() as st1:
            w_uq = sbt(k, st1, "w_uq", [128, 2, 1536], BF16)
            w_uqR = sbt(k, st1, "w_uqR", [128, 2, 8, 64], BF16)
            w_ukv = sbt(k, st1, "w_ukv", [128, 2048], BF16)
            wb = fw.buf("w_u")
            uq_ap = d["mla_w_uq"][0].rearrange("(kc p) f -> p kc f", p=128)
            fw.dma("pool", out=w_uq[:], in_=uq_ap, writes=[wb])
            for h in range(8):
                c0 = h * 192 + 128
                fw.dma("pool", out=w_uqR[:, :, h, 0:32], in_=uq_ap[:, :, c0 + 32:c0 + 64], writes=[wb])
                fw.dma("pool", out=w_uqR[:, :, h, 32:64], in_=uq_ap[:, :, c0:c0 + 32], writes=[wb])
            fw.dma("pool", out=w_ukv[:], in_=d["mla_w_ukv"][0], writes=[wb])
            cm = sbt(k, st1, "cm512", [128, 4, 512], BF16)
            cm_b = fw.buf("cm512")
            fw.dma("pool", out=cm[:], in_=d["cm512"], writes=[cm_b])
            qn = sbt(k, st1, "qn", [128, S], BF16)
            qr = sbt(k, st1, "qr", [64, S], BF16)
            kn = sbt(k, st1, "kn", [128, S], BF16)
            V = sbt(k, st1, "V", [128, 32, 128], BF16)
            hd_b = [fw.buf(f"hd{t}") for t in range(NT)]
            t1 = sbt(k, st1, "t1b", [64, 512], F32)
            t2 = sbt(k, st1, "t2b", [64, 512], F32)
            t_b = fw.buf("t12b")
            PT = [sbt(k, st1, "PT", [128, 512], BF16) for _ in range(3)]
            PT_b = fw.bufs(3, "PT")
            rden = sbt(k, st1, "rden", [128, 512], F32)
            rden_b = fw.buf("rden")
            ps, psb = k.ps, k.psb
            scale = float(192 ** -0.5)
            si = 0
            for h in range(8):
                for t in range(NT):
                    ts_ = slice(t * 512, (t + 1) * 512)
                    c0 = h * 192
                    for j in range(2):
                        fw.op("pe", lambda e: e.matmul(ps[0][:], lhsT=w_uq[:, j, c0:c0 + 128], rhs=cqn[:, j, ts_],
                                                       start=(j == 0), stop=(j == 1)),
                              reads=[wb, lat_b[t]], writes=[psb[0]], inc=(j == 1))
                    for j in range(2):
                        fw.op("pe", lambda e: e.matmul(ps[1][0:64, :], lhsT=w_uq[:, j, c0 + 128:c0 + 192], rhs=cqn[:, j, ts_],
                                                       start=(j == 0), stop=(j == 1)),
                              reads=[wb, lat_b[t]], writes=[psb[1]], inc=(j == 1))
                    for j in range(2):
                        fw.op("pe", lambda e: e.matmul(ps[2][0:64, :], lhsT=w_uqR[:, j, h, :], rhs=cqn[:, j, ts_],
                                                       start=(j == 0), stop=(j == 1)),
                              reads=[wb, lat_b[t]], writes=[psb[2]], inc=(j == 1))
                    fw.op("pe", lambda e: e.matmul(ps[7][:], lhsT=w_ukv[:, h * 256:h * 256 + 128], rhs=ckvn[:, ts_],
                                                   start=True, stop=True),
                          reads=[wb, lat_b[t]], writes=[psb[7]])
                    fw.op("act", lambda e: e.activation(out=qn[:, ts_], in_=ps[0][:], func=AF.Copy),
                          reads=[psb[0]], writes=[hd_b[t]])
                    fw.op("act", lambda e: e.activation(out=kn[:, ts_], in_=ps[7][:], func=AF.Copy),
                          reads=[psb[7]], writes=[hd_b[t]])
                    fw.op("dve", lambda e: e.tensor_tensor(out=t1[:], in0=ps[1][0:64, :], in1=cos64[:, ts_], op=ALU.mult),
                          reads=[psb[1], trig_b], writes=[t_b])
                    fw.op("dve", lambda e: e.tensor_tensor(out=t2[:], in0=ps[2][0:64, :], in1=sin64[:, ts_], op=ALU.mult),
                          reads=[psb[2], trig_b], writes=[t_b])
                    fw.op("dve", lambda e: e.tensor_tensor(out=qr[:, ts_], in0=t1[:], in1=t2[:], op=ALU.add),
                          reads=[t_b], writes=[hd_b[t]])
                    for sub in range(4):
                        tk = slice(t * 512 + sub * 128, t * 512 + (sub + 1) * 128)
                        fw.op("pe", lambda e: e.matmul(ps[3][:, sub * 128:(sub + 1) * 128], lhsT=ckvn[:, tk],
                                                       rhs=w_ukv[:, h * 256 + 128:h * 256 + 256], start=True, stop=True),
                              reads=[wb, lat_b[t]], writes=[psb[3]], inc=(sub == 3))
                    fw.op("act", lambda e: e.activation(out=V[:, t * 4:(t + 1) * 4, :],
                                                        in_=ps[3][:].rearrange("p (q c) -> p q c", q=4), func=AF.Copy),
                          reads=[psb[3]], writes=[hd_b[t]])
                tiles = [(Q, kt) for Q in range(NT) for kt in range(4 * Q + 4)]

                def emit_S(Q, kt, pi):
                    qs = slice(Q * 512, (Q + 1) * 512)
                    ks = slice(kt * 128, (kt + 1) * 128)
                    diag = kt >= 4 * Q
                    tq = kt // 4
                    fw.op("pe", lambda e: e.matmul(ps[pi][:], lhsT=kn[:, ks], rhs=qn[:, qs], start=True, stop=False),
                          reads=[hd_b[tq], hd_b[Q]], writes=[psb[pi]], inc=False)
                    fw.op("pe", lambda e: e.matmul(ps[pi][:], lhsT=krT[:, ks], rhs=qr[:, qs], start=False, stop=(not diag)),
                          reads=[lat_b[tq], hd_b[Q]], writes=[psb[pi]], inc=(not diag))
                    if diag:
                        fw.op("pe", lambda e: e.matmul(ps[pi][:], lhsT=k.ident_b[:], rhs=cm[:, kt - 4 * Q, :],
                                                       start=False, stop=True),
                              reads=[cm_b, k.const_b], writes=[psb[pi]])

                emit_S(tiles[0][0], tiles[0][1], si % 3)
                for ti_, (Q, kt) in enumerate(tiles):
                    qs = slice(Q * 512, (Q + 1) * 512)
                    po, pd = 3 + (Q % 2), 5 + (Q % 2)
                    nk = 4 * Q + 4
                    tq = kt // 4
                    pi = si % 3
                    si += 1
                    fw.op("act", lambda e: e.activation(out=PT[pi][:], in_=ps[pi][:], func=AF.Exp, scale=scale),
                          reads=[psb[pi]], writes=[PT_b[pi]])
                    if ti_ + 1 < len(tiles):
                        emit_S(tiles[ti_ + 1][0], tiles[ti_ + 1][1], si % 3)
                    fw.op("pe", lambda e: e.matmul(ps[po][:], lhsT=V[:, kt, :], rhs=PT[pi][:], start=(kt == 0), stop=(kt == nk - 1)),
                          reads=[hd_b[tq], PT_b[pi]], writes=[psb[po]], inc=False)
                    fw.op("pe", lambda e: e.matmul(ps[pd][:], lhsT=k.ones_b[:], rhs=PT[pi][:], start=(kt == 0), stop=(kt == nk - 1)),
                          reads=[k.const_b, PT_b[pi]], writes=[psb[pd]], inc=True)
                    if kt == nk - 1:
                        fw.op("dve", lambda e: e.reciprocal(out=rden[:], in_=ps[pd][:]), reads=[psb[pd]], writes=[rden_b])
                        fw.op("dve", lambda e: e.tensor_tensor(out=OT[:, h, qs], in0=ps[po][:], in1=rden[:], op=ALU.mult),
                              reads=[psb[po], rden_b], writes=[OT_b[h][Q]])
            fw.barrier(release=[wb, cm_b])
        if k.stop == "1b":
            k.dbg_OT = (OT, OT_b)
            dump_OT(k, OT, OT_b)
            return
        out_proj(k, OT, OT_b, d["mla_w_o"][0], V_MOD[0] + 16)


def dump_OT(k, OT, OT_b):
    fw = k.fw
    for h in range(8):
        flat = [b for b in OT_b[h]]
        fw.dma("pool", out=k.dbg[:, h, :], in_=OT[:, h, :], reads=flat)
    fw.barrier()


def out_proj(k, OT, OT_b, wo_ap, ga_col):
    nc, fw = k.nc, k.fw
    with contextlib.ExitStack() as st:
        w_o = sbt(k, st, "w_o", [128, 8, D], BF16)
        wb = fw.buf("w_o")
        fw.dma("pool", out=w_o[:], in_=wo_ap.rearrange("(kc p) f -> p kc f", p=128), writes=[wb])
        xTt = [sbt(k, st, "xTo", [128, 8, 512], F32) for _ in range(2)]
        xTt_b = [[fw.buf(f"xTo{i}_{dc}") for dc in range(8)] for i in range(2)]
        ps, psb = k.ps, k.psb
        pi = 0
        for t in range(NT):
            ts_ = slice(t * 512, (t + 1) * 512)
            i = t % 2
            fw.dma("sp", out=xTt[i][:], in_=k.xT_d[:, :, ts_], reads=[k.xT_db[t]], writes=xTt_b[i])
            for dc in range(8):
                p = pi % 4
                pi += 1
                for h in range(8):
                    fw.op("pe", lambda e: e.matmul(ps[p][:], lhsT=w_o[:, h, dc * 128:(dc + 1) * 128], rhs=OT[:, h, ts_],
                                                   start=(h == 0), stop=(h == 7)),
                          reads=[wb, OT_b[h][t]], writes=[psb[p]], inc=(h == 7))
                fw.op("dve", lambda e: e.scalar_tensor_tensor(out=xTt[i][:, dc, :], in0=ps[p][:], scalar=k.vec[:, ga_col + dc:ga_col + dc + 1],
                                                              in1=xTt[i][:, dc, :], op0=ALU.mult, op1=ALU.add),
                      reads=[psb[p], k.vec_b, xTt_b[i][dc]], writes=[xTt_b[i][dc]])
            fw.dma("sp", out=k.xT_d[:, :, ts_], in_=xTt[i][:], reads=xTt_b[i], writes=[k.xT_db[t]])
        fw.barrier(release=[wb] + [xTt_b[i][0] for i in range(2)])


def phase_moe(k, layer, n_exp=32, n_chunks=4):
    nc, fw, d = k.nc, k.fw, k.d
    TC = 1024
    ntt = TC // 512
    with contextlib.ExitStack() as st:
        W = [sbt(k, st, "moe_w", [128, 8, 1024], BF16) for _ in range(4)]
        Wb = fw.bufs(4, "moe_w")
        aT = sbt(k, st, "moe_aT", [128, 8, TC], BF16)
        ab = [[fw.buf(f"a{t}_{f}") for f in range(8)] for t in range(ntt)]
        gc = [sbt(k, st, "moe_gc", [128, 512], F32) for _ in range(2)]
        tt_ = [sbt(k, st, "moe_t", [128, 512], F32) for _ in range(2)]
        sg = [sbt(k, st, "moe_sg", [128, 512], BF16) for _ in range(2)]
        pp = [sbt(k, st, "moe_p", [128, 512], BF16) for _ in range(2)]
        p2 = [sbt(k, st, "moe_p2", [128, 512], BF16) for _ in range(2)]
        gbc = [sbt(k, st, "moe_gbc", [128, 512], BF16) for _ in range(2)]
        gcb, tb, sgb, pb, p2b, gbcb = (fw.bufs(2, n) for n in ("gc", "t", "sg", "p", "p2", "gbc"))
        hT = sbt(k, st, "moe_hT", [128, 8, TC], BF16)
        hb = fw.bufs(ntt, "moe_h")
        xT = sbt(k, st, "moe_xT", [128, 8, TC], F32)
        xb = [[fw.buf(f"mx{t}_{dc}") for dc in range(8)] for t in range(ntt)]
        gT = sbt(k, st, "moe_gT", [32, TC], BF16)
        gTb = fw.buf("moe_gT")
        sel = sbt(k, st, "moe_sel", [32, 32, 128], BF16)
        selb = fw.buf("moe_sel")
        fw.dma("pool", out=sel[:], in_=d["sel32"], writes=[selb])
        bgT = sbt(k, st, "moe_bgT", [128, 8, 32], F32)
        bu1T = sbt(k, st, "moe_buT", [128, 8, 32], F32)
        bb = fw.buf("moe_bias")
        bd = sbt(k, st, "moe_bd", [32, D], BF16)
        bdb = fw.buf("moe_bd")
        fw.dma("pool", out=bd[:], in_=d["b_down"][layer], writes=[bdb])
        rw = sbt(k, st, "moe_rw", [128, 8, 32], BF16)
        rb = sbt(k, st, "moe_rb", [1, 32], BF16)
        rwb = fw.buf("moe_rw")
        fw.dma("pool", out=rw[:], in_=d["router_w"][layer].rearrange("(kc p) e -> p kc e", p=128), writes=[rwb])
        fw.dma("pool", out=rb[:], in_=d["router_b"][layer:layer + 1, :], writes=[rwb])
        NR = norm_alloc(k, st, 512)
        ps, psb = k.ps, k.psb
        braw = sbt(k, st, "moe_braw", [32, 2, D], F32)
        brawb = fw.buf("moe_braw")
        fw.dma("sp", out=braw[:, 0, :], in_=d["b_gate"][layer], writes=[brawb])
        fw.dma("sp", out=braw[:, 1, :], in_=d["b_up"][layer], writes=[brawb])
        for which, dst in ((0, bgT), (1, bu1T)):
            for fc in range(8):
                fw.op("pe", lambda e: e.matmul(ps[7][:, fc * 32:(fc + 1) * 32], lhsT=braw[:, which, fc * 128:(fc + 1) * 128],
                                               rhs=k.ident_f[0:32, 0:32], start=True, stop=True),
                      reads=[brawb, k.const_b], writes=[psb[7]], inc=(fc == 7))
            if which == 0:
                fw.op("dve", lambda e: e.tensor_copy(out=dst[:], in_=ps[7][:, 0:256].rearrange("p (f e) -> p f e", f=8)),
                      reads=[psb[7]], writes=[bb])
            else:
                fw.op("dve", lambda e: e.tensor_scalar(out=dst[:], in0=ps[7][:, 0:256].rearrange("p (f e) -> p f e", f=8),
                                                       scalar1=1.0, scalar2=None, op0=ALU.add),
                      reads=[psb[7]], writes=[bb])
        lg = sbt(k, st, "moe_lg", [128, 32], F32)
        mx = sbt(k, st, "moe_mx", [128, 8], F32)
        nm0 = sbt(k, st, "moe_nm0", [128, 1], F32)
        ex = sbt(k, st, "moe_ex", [128, 32], F32)
        em = sbt(k, st, "moe_em", [128, 32], F32)
        ssum = sbt(k, st, "moe_ss", [128, 1], F32)
        gts = sbt(k, st, "moe_gts", [128, 32], BF16)
        rt_b = fw.buf("moe_rt")
        gf_col = V_MOD[layer] + 40
        mats = []
        for e_ in range(n_exp):
            mats += [d["w_gate"][layer, e_], d["w_up"][layer, e_], d["w_down"][layer, e_]]
        nm = len(mats)
        kk = [0, 0, 0]
        pend = []

        def load(m):
            slot = (k.wm + m) % 4
            fw.dma("pool", out=W[slot][:], in_=mats[m % nm].rearrange("(kc p) f -> p kc f", p=128), writes=[Wb[slot]])

        PG, PU, PY, PB = [0, 1], [2, 3], [4, 5], 6
        total = nm * n_chunks
        for m in range(min(4, total)):
            load(m)
        for c in range(n_chunks):
            c0 = c * TC
            for t in range(ntt):
                fw.dma("sp", out=xT[:, :, t * 512:(t + 1) * 512], in_=k.xT_d[:, :, c0 + t * 512:c0 + (t + 1) * 512],
                       reads=[k.xT_db[c * ntt + t]], writes=xb[t])
            for t in range(ntt):
                ts_ = slice(t * 512, (t + 1) * 512)
                xall = xb[t]
                fw.op("act", lambda e: e.activation(out=NR.sq[:], in_=xT[:, :, ts_], func=AF.Square), reads=xall, writes=[NR.sqb])
                for kc in range(8):
                    fw.op("pe", lambda e: e.matmul(ps[7][:], lhsT=k.ones_b[:], rhs=NR.sq[:, kc, :], start=(kc == 0), stop=(kc == 7)),
                          reads=[NR.sqb, k.const_b], writes=[psb[7]], inc=(kc == 7))
                fw.op("act", lambda e: e.activation(out=NR.rstd[:], in_=ps[7][:], func=AF.Sqrt, scale=1.0 / D, bias=k.eps_col[:]),
                      reads=[psb[7], k.const_b], writes=[NR.rstdb])
                fw.op("dve", lambda e: e.reciprocal(out=NR.rstd[:], in_=NR.rstd[:]), reads=[NR.rstdb], writes=[NR.rstdb])
                for kc in range(8):
                    i = NR.i % 2
                    NR.i += 1
                    fw.op("dve", lambda e: e.scalar_tensor_tensor(out=NR.tmp[i][:], in0=xT[:, kc, ts_],
                                                                  scalar=k.vec[:, V_GSF[layer] + kc:V_GSF[layer] + kc + 1],
                                                                  in1=NR.rstd[:], op0=ALU.mult, op1=ALU.mult),
                          reads=[xall[kc], NR.rstdb, k.vec_b], writes=[NR.tmpb[i]])
                    fw.op("act", lambda e: e.activation(out=hT[:, kc, ts_], in_=NR.tmp[i][:], func=AF.Identity,
                                                        bias=k.vec[:, V_MOD[layer] + 24 + kc:V_MOD[layer] + 24 + kc + 1]),
                          reads=[NR.tmpb[i], k.vec_b], writes=[hb[t]])
                for sub in range(4):
                    tk = slice(t * 512 + sub * 128, t * 512 + (sub + 1) * 128)
                    for kc in range(8):
                        fw.op("pe", lambda e: e.matmul(ps[7][:, 0:32], lhsT=hT[:, kc, tk], rhs=rw[:, kc, :], start=(kc == 0), stop=False),
                              reads=[hb[t], rwb], writes=[psb[7]], inc=False)
                    fw.op("pe", lambda e: e.matmul(ps[7][:, 0:32], lhsT=k.ones_b[0:1, :], rhs=rb[0:1, :], start=False, stop=True),
                          reads=[rwb, k.const_b], writes=[psb[7]])
                    fw.op("dve", lambda e: e.tensor_copy(out=lg[:], in_=ps[7][:, 0:32]), reads=[psb[7]], writes=[rt_b])
                    fw.op("dve", lambda e: e.max(out=mx[:], in_=lg[:]), reads=[rt_b], writes=[rt_b])
                    fw.op("dve", lambda e: e.tensor_scalar(out=nm0[:], in0=mx[:, 0:1], scalar1=-1.0, scalar2=None, op0=ALU.mult),
                          reads=[rt_b], writes=[rt_b])
                    fw.op("act", lambda e: e.activation(out=ex[:], in_=lg[:], func=AF.Exp, bias=nm0[:]),
                          reads=[rt_b], writes=[rt_b])
                    fw.op("dve", lambda e: e.scalar_tensor_tensor(out=em[:], in0=lg[:], scalar=mx[:, 3:4], in1=ex[:],
                                                                  op0=ALU.is_ge, op1=ALU.mult),
                          reads=[rt_b], writes=[rt_b])
                    fw.op("dve", lambda e: e.tensor_reduce(out=ssum[:], in_=em[:], axis=AX.X, op=ALU.add), reads=[rt_b], writes=[rt_b])
                    fw.op("dve", lambda e: e.reciprocal(out=ssum[:], in_=ssum[:]), reads=[rt_b], writes=[rt_b])
                    fw.op("dve", lambda e: e.tensor_scalar(out=gts[:], in0=em[:], scalar1=ssum[:, 0:1], scalar2=None, op0=ALU.mult),
                          reads=[rt_b], writes=[rt_b])
                    fw.op("pe", lambda e: e.matmul(ps[6][0:32, 0:128], lhsT=gts[:], rhs=k.ident_b[:], start=True, stop=True),
                          reads=[rt_b, k.const_b], writes=[psb[6]])
                    fw.op("act", lambda e: e.activation(out=gT[:, tk], in_=ps[6][0:32, 0:128], func=AF.Copy),
                          reads=[psb[6]], writes=[gTb])
            for e_ in range(n_exp):
                mbase = c * nm + 3 * e_
                sg_, su_, sd_ = [(k.wm + mbase + j) % 4 for j in range(3)]
                Wg, Wu, Wd = W[sg_], W[su_], W[sd_]
                Wgb, Wub, Wdb = Wb[sg_], Wb[su_], Wb[sd_]
                for t in range(ntt):
                    ts_ = slice(t * 512, (t + 1) * 512)
                    kg = kk[1] % 2
                    kk[1] += 1
                    fw.op("pe", lambda e: e.matmul(ps[PB][:], lhsT=sel[:, e_, :], rhs=gT[:, ts_], start=True, stop=True),
                          reads=[selb, gTb], writes=[psb[PB]])
                    fw.op("act", lambda e: e.activation(out=gbc[kg][:], in_=ps[PB][:], func=AF.Copy),
                          reads=[psb[PB]], writes=[gbcb[kg]])
                    for fc in range(8):
                        i = kk[0] % 2
                        kk[0] += 1
                        fs = slice(fc * 128, (fc + 1) * 128)
                        pg, pu = PG[i], PU[i]
                        for kc in range(8):
                            fw.op("pe", lambda e: e.matmul(ps[pg][:], lhsT=Wg[:, kc, fs], rhs=hT[:, kc, ts_], start=(kc == 0), stop=(kc == 7)),
                                  reads=[Wgb, hb[t]], writes=[psb[pg]], inc=(kc == 7))
                        for kc in range(8):
                            fw.op("pe", lambda e: e.matmul(ps[pu][:], lhsT=Wu[:, kc, fs], rhs=hT[:, kc, ts_], start=(kc == 0), stop=(kc == 7)),
                                  reads=[Wub, hb[t]], writes=[psb[pu]], inc=(kc == 7))
                        fw.op("dve", lambda e: e.tensor_scalar(out=gc[i][:], in0=ps[pg][:], scalar1=bgT[:, fc, e_:e_ + 1], scalar2=7.0,
                                                               op0=ALU.add, op1=ALU.min), reads=[psb[pg], bb], writes=[gcb[i]])
                        fw.op("act", lambda e: e.activation(out=sg[i][:], in_=gc[i][:], func=AF.Sigmoid, scale=1.702),
                              reads=[gcb[i]], writes=[sgb[i]])
                        fw.op("dve", lambda e: e.tensor_scalar(out=tt_[i][:], in0=ps[pu][:], scalar1=bu1T[:, fc, e_:e_ + 1], scalar2=-6.0,
                                                               op0=ALU.add, op1=ALU.max), reads=[psb[pu], bb], writes=[tb[i]])
                        fw.op("pool", lambda e: e.tensor_tensor(out=pp[i][:], in0=gc[i][:], in1=sg[i][:], op=ALU.mult),
                              reads=[gcb[i], sgb[i]], writes=[pb[i]])
                        fw.op("pool", lambda e: e.tensor_tensor(out=p2[i][:], in0=pp[i][:], in1=gbc[kg][:], op=ALU.mult),
                              reads=[pb[i], gbcb[kg]], writes=[p2b[i]])
                        if pend:
                            pend.pop()()

                        def _a(i=i, fc=fc, ts_=ts_, t=t):
                            fw.op("dve", lambda e: e.scalar_tensor_tensor(out=aT[:, fc, ts_], in0=tt_[i][:], scalar=8.0, in1=p2[i][:],
                                                                          op0=ALU.min, op1=ALU.mult),
                                  reads=[tb[i], p2b[i]], writes=[ab[t][fc]])
                        pend.append(_a)
                if pend:
                    pend.pop()()
                for m in (mbase + 4, mbase + 5):
                    if m < total:
                        load(m)
                for t in range(ntt):
                    ts_ = slice(t * 512, (t + 1) * 512)
                    for dc in range(8):
                        py = PY[kk[2] % 2]
                        kk[2] += 1
                        ds_ = slice(dc * 128, (dc + 1) * 128)
                        if e_ == 0:
                            fw.op("pe", lambda e: e.matmul(ps[py][:], lhsT=bd[:, ds_], rhs=gT[:, ts_], start=True, stop=False),
                                  reads=[bdb, gTb], writes=[psb[py]], inc=False)
                        for fc in range(8):
                            fw.op("pe", lambda e: e.matmul(ps[py][:], lhsT=Wd[:, fc, ds_], rhs=aT[:, fc, ts_],
                                                           start=(fc == 0 and e_ != 0), stop=(fc == 7)),
                                  reads=[Wdb, ab[t][fc]], writes=[psb[py]], inc=(fc == 7))
                        fw.op("dve", lambda e: e.scalar_tensor_tensor(out=xT[:, dc, ts_], in0=ps[py][:], scalar=k.vec[:, gf_col + dc:gf_col + dc + 1],
                                                                      in1=xT[:, dc, ts_], op0=ALU.mult, op1=ALU.add),
                              reads=[psb[py], k.vec_b, xb[t][dc]], writes=[xb[t][dc]])
                m = mbase + 6
                if m < total:
                    load(m)
            for t in range(ntt):
                fw.dma("sp", out=k.xT_d[:, :, c0 + t * 512:c0 + (t + 1) * 512], in_=xT[:, :, t * 512:(t + 1) * 512],
                       reads=xb[t], writes=[k.xT_db[c * ntt + t]])
        k.wm = (k.wm + total) % 4
        fw.barrier(release=Wb + [selb, bdb, rwb, brawb] + [xb[t][0] for t in range(ntt)])


TS = 512
NTL = 64
NROWS = NTL * TS


def phase_moe_sparse(k, layer):
    nc, fw, d = k.nc, k.fw, k.d
    ps, psb = k.ps, k.psb
    U32 = mybir.dt.uint32
    NSUB = S // 128
    Hs, Ys = k.Hs, k.Ys
    with contextlib.ExitStack() as st:
        ridx = sbt(k, st, "sp_ridx", [128, NSUB * 4], I32)
        g4 = sbt(k, st, "sp_g4", [128, NSUB * 4], F32)
        gT = sbt(k, st, "sp_gT", [32, S], BF16)
        idxw = sbt(k, st, "sp_idxw", [128, NTL * 8], I32)
        OH = sbt(k, st, "sp_OH", [32, NTL], F32)
        te_b = fw.buf("sp_te")
        pers_b = [fw.buf(f"sp_pers{i}") for i in range(NSUB)]
        gT_b = [fw.buf(f"sp_gT{i}") for i in range(NSUB)]
        with contextlib.ExitStack() as st1:
            rw = sbt(k, st1, "sp_rw", [128, 8, 32], BF16)
            rb = sbt(k, st1, "sp_rb", [1, 32], BF16)
            ltri = sbt(k, st1, "sp_ltri", [128, 128], BF16)
            iota = sbt(k, st1, "sp_iota", [128, 64], F32)
            cst_b = fw.buf("sp_cst")
            fw.dma("pool", out=rw[:], in_=d["router_w"][layer].rearrange("(kc p) e -> p kc e", p=128), writes=[cst_b])
            fw.dma("pool", out=rb[:], in_=d["router_b"][layer:layer + 1, :], writes=[cst_b])
            fw.dma("pool", out=ltri[:], in_=d["ltri"], writes=[cst_b])
            fw.dma("sp", out=iota[:], in_=d["iota64"], writes=[cst_b])
            macc = sbt(k, st1, "sp_macc", [128, 32], BF16)
            macc_b = fw.buf("sp_macc")
            fw.op("dve", lambda e: e.memset(macc[:], 0.0), writes=[macc_b])
            xTt = [sbt(k, st1, "sp_xT", [128, 8, 512], F32) for _ in range(2)]
            xTt_b = fw.bufs(2, "sp_xT")
            hTt = [sbt(k, st1, "sp_hT", [128, 8, 512], BF16) for _ in range(2)]
            hTt_b = fw.bufs(2, "sp_hT")
            NR = norm_alloc(k, st1, 512)
            htm = sbt(k, st1, "sp_htm", [128, NSUB, D], BF16)
            htm_b = fw.bufs(NSUB, "sp_htm")
            posk = sbt(k, st1, "sp_posk", [128, NSUB * 4], F32)
            efk = sbt(k, st1, "sp_efk", [128, NSUB * 4], F32)
            lg = sbt(k, st1, "sp_lg", [128, 32], F32)
            mx = sbt(k, st1, "sp_mx", [128, 8], F32)
            ix = sbt(k, st1, "sp_ix", [128, 8], U32)
            nm0 = sbt(k, st1, "sp_nm0", [128, 1], F32)
            ex = sbt(k, st1, "sp_ex", [128, 32], F32)
            em = sbt(k, st1, "sp_em", [128, 32], F32)
            ssum = sbt(k, st1, "sp_ss", [128, 1], F32)
            gts = sbt(k, st1, "sp_gts", [128, 32], BF16)
            msk = sbt(k, st1, "sp_msk", [128, 32], BF16)
            pos = sbt(k, st1, "sp_pos", [128, 32], F32)
            junk = sbt(k, st1, "sp_junk", [128, 32], F32)
            rr = sbt(k, st1, "sp_rr", [128, 4], F32)
            rt_b = fw.buf("sp_rt")
            hi = 0
            for t in range(NT):
                ts_ = slice(t * 512, (t + 1) * 512)
                i = t % 2
                fw.dma("sp", out=xTt[i][:], in_=k.xT_d[:, :, ts_], reads=[k.xT_db[t]], writes=[xTt_b[i]])
                norm_rstd(k, NR, xTt[i], xTt_b[i], slice(0, 512), 0)
                norm_apply(k, NR, xTt[i], xTt_b[i], slice(0, 512), V_GSF[layer], V_MOD[layer] + 24,
                           lambda kc: hTt[i][:, kc, :], hTt_b[i])
                for sub in range(4):
                    s_ = t * 4 + sub
                    c4 = slice(s_ * 4, s_ * 4 + 4)
                    tk = slice(sub * 128, (sub + 1) * 128)
                    for kc in range(8):
                        fw.op("pe", lambda e: e.matmul(ps[7][:, 0:32], lhsT=hTt[i][:, kc, tk], rhs=rw[:, kc, :], start=(kc == 0), stop=False),
                              reads=[hTt_b[i], cst_b], writes=[psb[7]], inc=False)
                    fw.op("pe", lambda e: e.matmul(ps[7][:, 0:32], lhsT=k.ones_b[0:1, :], rhs=rb[0:1, :], start=False, stop=True),
                          reads=[cst_b, k.const_b], writes=[psb[7]])
                    hb_ = hi % 2
                    hi += 1
                    psT = ps[1 + hb_][:].bitcast(BF16)
                    for kc in range(8):
                        fw.op("pe", lambda e: e.transpose(out=psT[:, kc * 128:(kc + 1) * 128], in_=hTt[i][:, kc, tk], identity=k.ident_b[:]),
                              reads=[hTt_b[i], k.const_b], writes=[psb[1 + hb_]], inc=(kc == 7))
                    fw.op("act", lambda e: e.activation(out=htm[:, s_, :], in_=psT, func=AF.Copy), reads=[psb[1 + hb_]], writes=[htm_b[s_]])
                    fw.op("dve", lambda e: e.tensor_copy(out=lg[:], in_=ps[7][:, 0:32]), reads=[psb[7]], writes=[rt_b])
                    fw.op("dve", lambda e: e.max(out=mx[:], in_=lg[:]), reads=[rt_b], writes=[rt_b])
                    fw.op("dve", lambda e: e.max_index(out=ix[:], in_max=mx[:], in_values=lg[:]), reads=[rt_b], writes=[rt_b])
                    fw.op("dve", lambda e: e.tensor_copy(out=efk[:, c4], in_=ix[:, 0:4]), reads=[rt_b], writes=[pers_b[s_]])
                    fw.op("dve", lambda e: e.tensor_scalar(out=nm0[:], in0=mx[:, 0:1], scalar1=-1.0, scalar2=None, op0=ALU.mult),
                          reads=[rt_b], writes=[rt_b])
                    fw.op("act", lambda e: e.activation(out=ex[:], in_=lg[:], func=AF.Exp, bias=nm0[:]), reads=[rt_b], writes=[rt_b])
                    fw.op("dve", lambda e: e.tensor_scalar(out=msk[:], in0=lg[:], scalar1=mx[:, 3:4], scalar2=None, op0=ALU.is_ge),
                          reads=[rt_b], writes=[rt_b])
                    fw.op("dve", lambda e: e.tensor_tensor(out=em[:], in0=msk[:], in1=ex[:], op=ALU.mult), reads=[rt_b], writes=[rt_b])
                    fw.op("dve", lambda e: e.tensor_reduce(out=ssum[:], in_=em[:], axis=AX.X, op=ALU.add), reads=[rt_b], writes=[rt_b])
                    fw.op("dve", lambda e: e.reciprocal(out=ssum[:], in_=ssum[:]), reads=[rt_b], writes=[rt_b])
                    fw.op("dve", lambda e: e.tensor_scalar(out=gts[:], in0=em[:], scalar1=ssum[:, 0:1], scalar2=None, op0=ALU.mult),
                          reads=[rt_b], writes=[rt_b])
                    fw.op("act", lambda e: e.activation(out=g4[:, c4], in_=mx[:, 0:4], func=AF.Exp, bias=nm0[:]),
                          reads=[rt_b], writes=[pers_b[s_]])
                    fw.op("dve", lambda e: e.tensor_scalar(out=g4[:, c4], in0=g4[:, c4], scalar1=ssum[:, 0:1], scalar2=None, op0=ALU.mult),
                          reads=[rt_b, pers_b[s_]], writes=[pers_b[s_]])
                    fw.op("pe", lambda e: e.matmul(ps[6][0:32, 0:128], lhsT=gts[:], rhs=k.ident_b[:], start=True, stop=True),
                          reads=[rt_b, k.const_b], writes=[psb[6]])
                    fw.op("act", lambda e: e.activation(out=gT[:, s_ * 128:(s_ + 1) * 128], in_=ps[6][0:32, 0:128], func=AF.Copy),
                          reads=[psb[6]], writes=[gT_b[s_]])
                    fw.op("pe", lambda e: e.matmul(ps[5][:, 0:32], lhsT=ltri[:], rhs=msk[:], start=True, stop=False),
                          reads=[cst_b, rt_b], writes=[psb[5]], inc=False)
                    fw.op("pe", lambda e: e.matmul(ps[5][:, 0:32], lhsT=k.ones_b[:], rhs=macc[:], start=False, stop=True),
                          reads=[k.const_b, macc_b], writes=[psb[5]])
                    fw.op("dve", lambda e: e.tensor_copy(out=pos[:], in_=ps[5][:, 0:32]), reads=[psb[5]], writes=[rt_b])
                    fw.op("dve", lambda e: e.tensor_tensor(out=macc[:], in0=macc[:], in1=msk[:], op=ALU.add),
                          reads=[rt_b, macc_b], writes=[macc_b])
                    for kk_ in range(4):
                        cc = s_ * 4 + kk_
                        fw.op("dve", lambda e: e.scalar_tensor_tensor(out=junk[:], in0=iota[:, 0:32], scalar=efk[:, cc:cc + 1], in1=pos[:],
                                                                      op0=ALU.is_equal, op1=ALU.mult, accum_out=posk[:, cc:cc + 1]),
                              reads=[rt_b, cst_b, pers_b[s_]], writes=[pers_b[s_]])
            cnt = sbt(k, st1, "sp_cnt", [128, 32], F32)
            ntl = sbt(k, st1, "sp_ntl", [128, 32], F32)
            sc = [sbt(k, st1, "sp_sc", [128, 32], F32) for _ in range(2)]
            segs = sbt(k, st1, "sp_segs", [128, 32], F32)
            inclT = sbt(k, st1, "sp_inclT", [32, 1], F32)
            cmp_ = sbt(k, st1, "sp_cmp", [32, NTL], F32)
            ones32 = sbt(k, st1, "sp_ones32", [32, 1], F32)
            tef = sbt(k, st1, "sp_tef", [1, NTL], F32)
            sg_b = fw.buf("sp_seg")
            fw.op("pe", lambda e: e.matmul(ps[5][:, 0:32], lhsT=k.ones_b[:], rhs=macc[:], start=True, stop=True),
                  reads=[k.const_b, macc_b], writes=[psb[5]])
            fw.op("dve", lambda e: e.tensor_copy(out=cnt[:], in_=ps[5][:, 0:32]), reads=[psb[5]], writes=[sg_b])
            fw.op("dve", lambda e: e.memset(ntl[:], 0.0), reads=[], writes=[sg_b])
            for j in range(S // TS):
                fw.op("dve", lambda e: e.scalar_tensor_tensor(out=ntl[:], in0=cnt[:], scalar=float(TS * j), in1=ntl[:],
                                                              op0=ALU.is_gt, op1=ALU.add), reads=[sg_b], writes=[sg_b])
            fw.op("dve", lambda e: e.tensor_copy(out=sc[0][:], in_=ntl[:]), reads=[sg_b], writes=[sg_b])
            cur = 0
            for sh in (1, 2, 4, 8, 16):
                nxt = 1 - cur
                fw.op("dve", lambda e: e.tensor_copy(out=sc[nxt][:, 0:sh], in_=sc[cur][:, 0:sh]), reads=[sg_b], writes=[sg_b])
                fw.op("dve", lambda e: e.tensor_tensor(out=sc[nxt][:, sh:32], in0=sc[cur][:, sh:32], in1=sc[cur][:, 0:32 - sh], op=ALU.add),
                      reads=[sg_b], writes=[sg_b])
                cur = nxt
            incl = sc[cur]
            fw.op("dve", lambda e: e.tensor_tensor(out=segs[:], in0=incl[:], in1=ntl[:], op=ALU.subtract), reads=[sg_b], writes=[sg_b])
            fw.op("dve", lambda e: e.tensor_scalar(out=segs[:], in0=segs[:], scalar1=float(TS), scalar2=None, op0=ALU.mult),
                  reads=[sg_b], writes=[sg_b])
            fw.op("pe", lambda e: e.matmul(ps[5][0:32, 0:1], lhsT=incl[0:1, :], rhs=k.ones_row[0:1, 0:1], start=True, stop=True),
                  reads=[sg_b, k.const_b], writes=[psb[5]])
            fw.op("dve", lambda e: e.tensor_copy(out=inclT[:], in_=ps[5][0:32, 0:1]), reads=[psb[5]], writes=[sg_b])
            fw.op("dve", lambda e: e.tensor_scalar(out=cmp_[:], in0=iota[0:32, 0:NTL], scalar1=inclT[:, 0:1], scalar2=None, op0=ALU.is_ge),
                  reads=[sg_b, cst_b], writes=[sg_b])
            fw.op("dve", lambda e: e.memset(ones32[:], 1.0), reads=[], writes=[sg_b])
            fw.op("pe", lambda e: e.matmul(ps[5][0:1, 0:NTL], lhsT=ones32[:], rhs=cmp_[:], start=True, stop=True),
                  reads=[sg_b], writes=[psb[5]])
            tailf = sbt(k, st1, "sp_tailf", [1, NTL], F32)
            fw.op("dve", lambda e: e.tensor_scalar(out=tailf[:], in0=ps[5][0:1, 0:NTL], scalar1=32.0, scalar2=1.0e6, op0=ALU.is_ge, op1=ALU.mult),
                  reads=[psb[5]], writes=[sg_b])
            fw.op("dve", lambda e: e.tensor_scalar(out=tef[:], in0=ps[5][0:1, 0:NTL], scalar1=31.0, scalar2=None, op0=ALU.min),
                  reads=[psb[5]], writes=[sg_b])
            tebc = sbt(k, st1, "sp_tebc", [128, NTL], F32)
            idxf = sbt(k, st1, "sp_idxf", [128, NTL * 8], F32)
            pk = sbt(k, st1, "sp_pk", [128, 9], F32)
            fw.dma("sp", out=pk[:], in_=d["pk9"], writes=[sg_b])
            fw.op("pe", lambda e: e.matmul(ps[5][:, 0:NTL], lhsT=k.ones_row[0:1, :], rhs=tef[0:1, :], start=True, stop=True),
                  reads=[sg_b, k.const_b], writes=[psb[5]])
            fw.op("dve", lambda e: e.tensor_copy(out=tebc[:], in_=ps[5][:, 0:NTL]), reads=[psb[5]], writes=[sg_b])
            tebl = sbt(k, st1, "sp_tebl", [128, NTL], F32)
            fw.op("pe", lambda e: e.matmul(ps[5][:, 0:NTL], lhsT=k.ones_row[0:1, :], rhs=tailf[0:1, :], start=True, stop=True),
                  reads=[sg_b, k.const_b], writes=[psb[5]])
            fw.op("dve", lambda e: e.scalar_tensor_tensor(out=tebl[:], in0=tebc[:], scalar=float(32 * layer), in1=ps[5][:, 0:NTL],
                                                          op0=ALU.add, op1=ALU.add), reads=[sg_b, psb[5]], writes=[sg_b])
            for kc in range(8):
                fw.op("dve", lambda e: e.tensor_scalar(out=idxf[:].rearrange("p (i c) -> p i c", c=8)[:, :, kc], in0=tebl[:],
                                                       scalar1=1024.0, scalar2=pk[:, kc:kc + 1], op0=ALU.mult, op1=ALU.add),
                      reads=[sg_b], writes=[sg_b])
            fw.op("dve", lambda e: e.tensor_copy(out=idxw[:], in_=idxf[:]), reads=[sg_b], writes=[te_b])
            fw.op("dve", lambda e: e.tensor_scalar(out=OH[:], in0=tebc[0:32, :], scalar1=pk[0:32, 8:9], scalar2=None, op0=ALU.is_equal),
                  reads=[sg_b], writes=[te_b])
            for s_ in range(NSUB):
                c4 = slice(s_ * 4, s_ * 4 + 4)
                for kk_ in range(4):
                    cc = s_ * 4 + kk_
                    fw.op("dve", lambda e: e.scalar_tensor_tensor(out=junk[:], in0=iota[:, 0:32], scalar=efk[:, cc:cc + 1], in1=segs[:],
                                                                  op0=ALU.is_equal, op1=ALU.mult, accum_out=rr[:, kk_:kk_ + 1]),
                          reads=[rt_b, cst_b, pers_b[s_], sg_b], writes=[rt_b])
                fw.op("dve", lambda e: e.tensor_tensor(out=rr[:], in0=rr[:], in1=posk[:, c4], op=ALU.add), reads=[rt_b, pers_b[s_]], writes=[rt_b])
                fw.op("dve", lambda e: e.tensor_copy(out=ridx[:, c4], in_=rr[:]), reads=[rt_b], writes=[pers_b[s_]])
                for kk_ in range(4):
                    cc = s_ * 4 + kk_
                    fw.dma_custom("pool", lambda e: e.indirect_dma_start(
                        out=Hs[:, :], out_offset=bass.IndirectOffsetOnAxis(ap=ridx[:, cc:cc + 1], axis=0),
                        in_=htm[:, s_, :], in_offset=None, bounds_check=k.bc_reg, oob_is_err=False),
                        reads=[htm_b[s_], pers_b[s_]], writes=[], sem_owner=htm_b[s_ % 8])
            fw.barrier(release=[cst_b] + xTt_b + htm_b[0:8])
        with contextlib.ExitStack() as st1:
            NW = 6
            W = [sbt(k, st1, "sp_w", [128, 8, 1024], BF16) for _ in range(NW)]
            Wb = fw.bufs(NW, "sp_w")
            Wkc = [fw.bufs(8, f"sp_w{i}_") for i in range(NW)]
            ball = sbt(k, st1, "sp_ball", [32, 2, D], BF16)
            ball_b = fw.buf("sp_ball")
            fw.dma("pool", out=ball[:, 0, :], in_=d["b_gate"][layer], writes=[ball_b])
            fw.dma("pool", out=ball[:, 1, :], in_=d["b_up"][layer], writes=[ball_b])
            ones32r = sbt(k, st1, "sp_ones32r", [32, TS], F32)
            fw.op("dve", lambda e: e.memset(ones32r[:], 1.0), writes=[ball_b])
            ohx = [sbt(k, st1, "sp_ohx", [32, TS], BF16) for _ in range(2)]
            ohx_b = fw.bufs(2, "sp_ohx")
            hs = [sbt(k, st1, "sp_hs", [128, 4, D], BF16) for _ in range(2)]
            hs_b = fw.bufs(2, "sp_hs")
            hTe = [sbt(k, st1, "sp_hTe", [128, 8, TS], BF16) for _ in range(2)]
            hTe_b = fw.bufs(2, "sp_hTe")
            aT = [sbt(k, st1, "sp_aT", [128, 8, TS], BF16) for _ in range(2)]
            a_b = [[fw.buf(f"sp_a{i}_{f}") for f in range(8)] for i in range(2)]
            gc = [sbt(k, st1, "sp_gc", [128, TS], F32) for _ in range(2)]
            tt_ = [sbt(k, st1, "sp_t", [128, TS], F32) for _ in range(2)]
            sg = [sbt(k, st1, "sp_sg", [128, TS], BF16) for _ in range(2)]
            pp = [sbt(k, st1, "sp_p", [128, TS], BF16) for _ in range(2)]
            gcb, tb, sgb, pb = (fw.bufs(2, n) for n in ("sp_gc", "sp_t", "sp_sg", "sp_p"))
            yt = sbt(k, st1, "sp_yt", [128, 4, D], F32)
            yt_b = fw.bufs(4, "sp_yt")
            wsrc = [d[nm_].rearrange("l e k f -> (l e k) f") for nm_ in ("w_gate", "w_up", "w_down")]

            def load_tile(i):
                for j in range(3):
                    slot = (3 * i + j) % NW
                    for kc in range(8):
                        cc = i * 8 + kc
                        fw.dma_custom("pool", lambda e: e.indirect_dma_start(
                            out=W[slot][:, kc, :], out_offset=None, in_=wsrc[j],
                            in_offset=bass.IndirectOffsetOnAxis(ap=idxw[:, cc:cc + 1], axis=0),
                            bounds_check=k.bcw_reg, oob_is_err=False),
                            reads=[te_b], writes=[Wkc[slot][kc]], sem_owner=Wb[slot])
                fw.op("dve", lambda e: e.tensor_scalar(out=ohx[i % 2][:], in0=ones32r[:], scalar1=OH[:, i:i + 1], scalar2=None, op0=ALU.mult),
                      reads=[te_b, ball_b], writes=[ohx_b[i % 2]])

            load_tile(0)
            load_tile(1)
            gi = 0
            pend = []
            for i in range(NTL):
                b_ = i % 2
                sl = [(3 * i + j) % NW for j in range(3)]
                Wg, Wu, Wd = W[sl[0]], W[sl[1]], W[sl[2]]
                Wgb, Wub, Wdb = Wkc[sl[0]], Wkc[sl[1]], Wkc[sl[2]]
                r0 = i * TS
                fw.dma("sp", out=hs[b_][:], in_=Hs[r0:r0 + TS, :].rearrange("(s p) d -> p s d", p=128), writes=[hs_b[b_]])
                for q in range(4):
                    pbk = 6 + q % 2
                    psT = ps[pbk][:].bitcast(BF16)
                    for k2 in range(2):
                        kc = q * 2 + k2
                        for sub in range(4):
                            c0 = k2 * TS + sub * 128
                            fw.op("pe", lambda e: e.transpose(out=psT[:, c0:c0 + 128], in_=hs[b_][:, sub, kc * 128:(kc + 1) * 128],
                                                              identity=k.ident_b[:]),
                                  reads=[hs_b[b_], k.const_b], writes=[psb[pbk]], inc=(k2 == 1 and sub == 3))
                    fw.op("act", lambda e: e.activation(out=hTe[b_][:, q * 2:q * 2 + 2, :],
                                                        in_=psT.rearrange("p (q c) -> p q c", q=2), func=AF.Copy),
                          reads=[psb[pbk]], writes=[hTe_b[b_]])
                for fc in range(8):
                    ii = gi % 2
                    gi += 1
                    fs = slice(fc * 128, (fc + 1) * 128)
                    pg, pu = ii, 2 + ii
                    for (pq, Wx, Wxb, jj) in ((pg, Wg, Wgb, 0), (pu, Wu, Wub, 1)):
                        for kc in range(8):
                            fw.op("pe", lambda e: e.matmul(ps[pq][:], lhsT=Wx[:, kc, fs], rhs=hTe[b_][:, kc, :], start=(kc == 0), stop=False),
                                  reads=Wxb + [hTe_b[b_]], writes=[psb[pq]], inc=False)
                        fw.op("pe", lambda e: e.matmul(ps[pq][:], lhsT=ball[:, jj, fs], rhs=ohx[b_][:], start=False, stop=True),
                              reads=[ball_b, ohx_b[b_]], writes=[psb[pq]])
                    fw.op("dve", lambda e: e.tensor_scalar(out=gc[ii][:], in0=ps[pg][:], scalar1=7.0, scalar2=None, op0=ALU.min),
                          reads=[psb[pg]], writes=[gcb[ii]])
                    fw.op("act", lambda e: e.activation(out=sg[ii][:], in_=gc[ii][:], func=AF.Sigmoid, scale=1.702),
                          reads=[gcb[ii]], writes=[sgb[ii]])
                    fw.op("dve", lambda e: e.tensor_scalar(out=tt_[ii][:], in0=ps[pu][:], scalar1=1.0, scalar2=-6.0,
                                                           op0=ALU.add, op1=ALU.max), reads=[psb[pu]], writes=[tb[ii]])
                    fw.op("pool", lambda e: e.tensor_tensor(out=pp[ii][:], in0=gc[ii][:], in1=sg[ii][:], op=ALU.mult),
                          reads=[gcb[ii], sgb[ii]], writes=[pb[ii]])
                    if pend:
                        pend.pop()()

                    def _a(ii=ii, fc=fc, b_=b_):
                        fw.op("dve", lambda e: e.scalar_tensor_tensor(out=aT[b_][:, fc, :], in0=tt_[ii][:], scalar=8.0, in1=pp[ii][:],
                                                                      op0=ALU.min, op1=ALU.mult),
                              reads=[tb[ii], pb[ii]], writes=[a_b[b_][fc]])
                    pend.append(_a)
                if pend:
                    pend.pop()()
                for sub in range(4):
                    for half in range(2):
                        py = 4 + half
                        for fc in range(8):
                            fw.op("pe", lambda e: e.matmul(ps[py][:], lhsT=aT[b_][:, fc, sub * 128:(sub + 1) * 128],
                                                           rhs=Wd[:, fc, half * 512:(half + 1) * 512], start=(fc == 0), stop=(fc == 7)),
                                  reads=Wdb + [a_b[b_][fc]], writes=[psb[py]], inc=(fc == 7))
                        if half == 0:
                            fw.op("act", lambda e: e.activation(out=yt[:, sub, 0:512], in_=ps[py][:], func=AF.Copy),
                                  reads=[psb[py]], writes=[yt_b[sub]])
                        else:
                            fw.op("dve", lambda e: e.tensor_copy(out=yt[:, sub, 512:1024], in_=ps[py][:]),
                                  reads=[psb[py]], writes=[yt_b[sub]])
                    fw.dma("sp", out=Ys[r0 + sub * 128:r0 + (sub + 1) * 128, :], in_=yt[:, sub, :],
                           reads=[yt_b[sub]], writes=[], sem_owner=yt_b[sub])
                if i + 2 < NTL:
                    load_tile(i + 2)
            fw.barrier(release=Wb + [ball_b] + hs_b + yt_b)
        with contextlib.ExitStack() as st1:
            bd = sbt(k, st1, "sp_bd", [32, D], BF16)
            bdb = fw.buf("sp_bd")
            fw.dma("pool", out=bd[:], in_=d["b_down"][layer], writes=[bdb])
            yg = [sbt(k, st1, "sp_yg", [128, 4, D], F32) for _ in range(2)]
            yg_b = [[fw.buf(f"sp_yg{i}_{q}") for q in range(4)] for i in range(2)]
            ftm = sbt(k, st1, "sp_ftm", [128, 4, D], F32)
            ftm_b = fw.bufs(4, "sp_ftm")
            xTt = [sbt(k, st1, "sp_xTc", [128, 8, 512], F32) for _ in range(2)]
            xTt_b = [[fw.buf(f"sp_xTc{i}_{dc}") for dc in range(8)] for i in range(2)]
            gf_col = V_MOD[layer] + 40
            gi = 0
            for t in range(NT):
                ts_ = slice(t * 512, (t + 1) * 512)
                i = t % 2
                fw.dma("sp", out=xTt[i][:], in_=k.xT_d[:, :, ts_], reads=[k.xT_db[t]], writes=xTt_b[i])
                for sub in range(4):
                    s_ = t * 4 + sub
                    g_ = gi % 2
                    gi += 1
                    for kk_ in range(4):
                        cc = s_ * 4 + kk_
                        fw.dma_custom("pool", lambda e: e.indirect_dma_start(
                            out=yg[g_][:, kk_, :], out_offset=None, in_=Ys[:, :],
                            in_offset=bass.IndirectOffsetOnAxis(ap=ridx[:, cc:cc + 1], axis=0),
                            bounds_check=k.bc_reg, oob_is_err=False),
                            reads=[pers_b[s_]], writes=[yg_b[g_][kk_]])
                    for half in range(2):
                        fw.op("pe", lambda e: e.matmul(ps[half][:], lhsT=gT[:, s_ * 128:(s_ + 1) * 128], rhs=bd[:, half * 512:(half + 1) * 512],
                                                       start=True, stop=True), reads=[gT_b[s_], bdb], writes=[psb[half]])
                        fw.op("dve", lambda e: e.scalar_tensor_tensor(out=ftm[:, sub, half * 512:(half + 1) * 512],
                                                                      in0=yg[g_][:, 0, half * 512:(half + 1) * 512], scalar=g4[:, s_ * 4:s_ * 4 + 1],
                                                                      in1=ps[half][:], op0=ALU.mult, op1=ALU.add),
                              reads=[yg_b[g_][0], pers_b[s_], psb[half]], writes=[ftm_b[sub]])
                    for kk_ in range(1, 4):
                        cc = s_ * 4 + kk_
                        fw.op("dve", lambda e: e.scalar_tensor_tensor(out=ftm[:, sub, :], in0=yg[g_][:, kk_, :], scalar=g4[:, cc:cc + 1],
                                                                      in1=ftm[:, sub, :], op0=ALU.mult, op1=ALU.add),
                              reads=[yg_b[g_][kk_], pers_b[s_], ftm_b[sub]], writes=[ftm_b[sub]])
                for dc in range(8):
                    pbk = 2 + dc % 4
                    for sub in range(4):
                        fw.op("pe", lambda e: e.transpose(out=ps[pbk][:, sub * 128:(sub + 1) * 128], in_=ftm[:, sub, dc * 128:(dc + 1) * 128],
                                                          identity=k.ident_f[:]),
                              reads=[ftm_b[sub], k.const_b], writes=[psb[pbk]], inc=(sub == 3))
                    fw.op("dve", lambda e: e.scalar_tensor_tensor(out=xTt[i][:, dc, :], in0=ps[pbk][:], scalar=k.vec[:, gf_col + dc:gf_col + dc + 1],
                                                                  in1=xTt[i][:, dc, :], op0=ALU.mult, op1=ALU.add),
                          reads=[psb[pbk], k.vec_b, xTt_b[i][dc]], writes=[xTt_b[i][dc]])
                fw.dma("sp", out=k.xT_d[:, :, ts_], in_=xTt[i][:], reads=xTt_b[i], writes=[k.xT_db[t]])
            fw.barrier(release=[bdb] + [yg_b[i][q] for i in range(2) for q in range(4)] + [xTt_b[i][0] for i in range(2)])

def phase_final(k):
    nc, fw, d = k.nc, k.fw, k.d
    with contextlib.ExitStack() as st:
        NR = norm_alloc(k, st, 512)
        xTt = [sbt(k, st, "fx", [128, 8, 512], F32) for _ in range(2)]
        xTt_b = fw.bufs(2, "fx")
        yT = sbt(k, st, "fy", [128, 8, 512], F32)
        yT_b = fw.buf("fy")
        yo = [sbt(k, st, "fyo", [128, D], F32) for _ in range(2)]
        yo_b = fw.bufs(2, "fyo")
        ps, psb = k.ps, k.psb
        oi = 0
        for t in range(NT):
            ts_ = slice(t * 512, (t + 1) * 512)
            i = t % 2
            fw.dma("sp", out=xTt[i][:], in_=k.xT_d[:, :, ts_], reads=[k.xT_db[t]], writes=[xTt_b[i]])
            norm_rstd(k, NR, xTt[i], xTt_b[i], slice(0, 512), 0)
            norm_apply(k, NR, xTt[i], xTt_b[i], slice(0, 512), V_GSFIN, V_FINMOD, lambda kc: yT[:, kc, :], yT_b)
            for sub in range(4):
                o = oi % 2
                oi += 1
                for half in range(2):
                    pi = 1 + (oi * 2 + half) % 4
                    for q in range(4):
                        kc = half * 4 + q
                        fw.op("pe", lambda e: e.transpose(out=ps[pi][:, q * 128:(q + 1) * 128],
                                                          in_=yT[:, kc, sub * 128:(sub + 1) * 128], identity=k.ident_f[:]),
                              reads=[yT_b, k.const_b], writes=[psb[pi]], inc=(q == 3))
                    fw.op("act" if half == 0 else "dve",
                          (lambda e: e.activation(out=yo[o][:, half * 512:(half + 1) * 512], in_=ps[pi][:], func=AF.Copy)) if half == 0 else
                          (lambda e: e.tensor_copy(out=yo[o][:, half * 512:(half + 1) * 512], in_=ps[pi][:])),
                          reads=[psb[pi]], writes=[yo_b[o]])
                r0 = t * 512 + sub * 128
                fw.dma("sp", out=d["out"][r0:r0 + 128, :], in_=yo[o][:], reads=[yo_b[o]], writes=[k.out_b])
        fw.barrier()


INPUT_NAMES = ["ada_w", "ada_b", "norm_attn_g", "norm_ffn_g", "mla_w_in", "mla_q_norm_g", "mla_w_uq", "mla_kv_norm_g",
               "mla_w_ukv", "mla_w_o", "kv_ada_w", "kv_ada_b", "kv_norm_g", "moba_w_kv", "moba_w_q", "moba_w_o",
               "router_w", "router_b", "w_gate", "b_gate", "w_up", "b_up", "w_down", "b_down",
               "final_ada_w", "final_ada_b", "final_norm_g"]
INPUT_SHAPES = {
    "ada_w": [2, 1024, 6144], "ada_b": [2, 6144], "norm_attn_g": [2, 1024], "norm_ffn_g": [2, 1024],
    "mla_w_in": [1, 1024, 448], "mla_q_norm_g": [1, 256], "mla_w_uq": [1, 256, 1536], "mla_kv_norm_g": [1, 128],
    "mla_w_ukv": [1, 128, 2048], "mla_w_o": [1, 1024, 1024], "kv_ada_w": [1024, 2048], "kv_ada_b": [1, 2048],
    "kv_norm_g": [1, 1024], "moba_w_kv": [1024, 2048], "moba_w_q": [1, 1024, 1024], "moba_w_o": [1, 1024, 1024],
    "router_w": [2, 1024, 32], "router_b": [2, 32], "w_gate": [2, 32, 1024, 1024], "b_gate": [2, 32, 1024],
    "w_up": [2, 32, 1024, 1024], "b_up": [2, 32, 1024], "w_down": [2, 32, 1024, 1024], "b_down": [2, 32, 1024],
    "final_ada_w": [1024, 2048], "final_ada_b": [1, 2048], "final_norm_g": [1, 1024],
}


def make_consts():
    c = {}
    c["ident"] = np.eye(128, dtype=np.float32)
    cv = np.zeros((128, 8), np.float32)
    j = np.arange(128)
    cv[:, 0] = (10000.0 ** (-(j % 32).astype(np.float32) * np.float32(2.0 / 64))).astype(np.float32)
    cv[:, 1] = (10000.0 ** (-(j % 64).astype(np.float32) * np.float32(2.0 / 128))).astype(np.float32)
    cv[:, 2] = np.where((j % 64) < 32, -1.0, 1.0)
    cv[:, 3] = np.where(j < 64, -1.0, 1.0)
    c["cvec"] = cv
    kp = np.arange(128)[:, None, None]
    jj = np.arange(4)[None, :, None]
    qf = np.arange(512)[None, None, :]
    c["cm512"] = np.where(128 * jj + kp <= qf, 0.0, NEG).astype(np.float32)
    jj2 = np.arange(2)[None, :, None]
    qf2 = np.arange(256)[None, None, :]
    c["cm256"] = np.where(128 * jj2 + kp <= qf2, 0.0, NEG).astype(np.float32)
    sel = np.zeros((32, 32, 128), np.float32)
    for e in range(32):
        sel[e, e, :] = 1.0
    c["sel32"] = sel
    oh = np.zeros((16, 16, 128), np.float32)
    for e in range(16):
        oh[e, e, :] = 1.0
    c["oh16"] = oh
    own = np.arange(16)[None, :, None]
    n = np.arange(16)[None, None, :]
    c["pastmask"] = np.broadcast_to(np.where(n < own, 0.0, -1e30), (128, 16, 16)).astype(np.float32).copy()
    c["valid01"] = np.broadcast_to(np.where(n < own, 1.0, 0.0), (128, 16, 16)).astype(np.float32).copy()
    c["iota64"] = np.broadcast_to(np.arange(64, dtype=np.float32)[None, :], (128, 64)).copy()
    pk = np.zeros((128, 9), np.float32)
    for kc in range(8):
        pk[:, kc] = kc * 128 + np.arange(128)
    pk[:, 8] = np.arange(128)
    c["pk9"] = pk
    tp = np.arange(128)
    c["ltri"] = (tp[:, None] < tp[None, :]).astype(np.float32)
    return c


CONST_SHAPES = {"ident": [128, 128], "cvec": [128, 8], "cm512": [128, 4, 512], "cm256": [128, 2, 256],
                "sel32": [32, 32, 128], "oh16": [16, 16, 128], "pastmask": [128, 16, 16], "valid01": [128, 16, 16],
                "iota64": [128, 64], "ltri": [128, 128], "pk9": [128, 9]}


def build_program(stop=None, n_exp=32, n_chunks=4, debug=False, sparse=True):
    nc = bass.Bass("TRN2", target_bir_lowering=False)
    k = K()
    k.nc = nc
    k.stop = stop
    k.wm = 0
    d = {}
    d["x"] = nc.dram_tensor("x", [S, D], F32, kind="ExternalInput").ap()
    d["cT"] = nc.dram_tensor("cT", [128, 8], F32, kind="ExternalInput").ap()
    d["pos"] = nc.dram_tensor("pos", [1, S], I32, kind="ExternalInput").ap()
    for n_ in INPUT_NAMES:
        d[n_] = nc.dram_tensor(n_, INPUT_SHAPES[n_], F32, kind="ExternalInput").ap()
    for n_, shp in CONST_SHAPES.items():
        d[n_] = nc.dram_tensor(n_, shp, F32, kind="ExternalInput").ap()
    d["out"] = nc.dram_tensor("out", [S, D], F32, kind="ExternalOutput").ap()
    k.d = d
    k.xT_d = nc.dram_tensor("xT_scr", [128, 8, S], F32, kind="Internal").ap()
    k.Hs = nc.dram_tensor("Hs_scr", [NROWS, D], BF16, kind="Internal").ap()
    k.Ys = nc.dram_tensor("Ys_scr", [NROWS, D], F32, kind="Internal").ap()
    k.wm6 = 0
    k.bc_reg = nc.gpsimd.to_reg(NROWS - 1)
    k.bcw_reg = nc.gpsimd.to_reg(2 * 32 * 1024 - 1)
    if debug:
        k.dbg = nc.dram_tensor("dbg", [128, 8, S], F32, kind="ExternalOutput").ap()
    with contextlib.ExitStack() as top:
        fw = FW(nc, top)
        k.fw = fw
        k.xT_db = fw.bufs(NT, "xTd")
        k.out_b = fw.buf("out")
        k.ps = [top.enter_context(nc.psum_tensor(f"ps{i}", [128, 512], F32)) for i in range(8)]
        k.psb = fw.bufs(8, "ps")
        k.vec = sbt(k, top, "vec", [128, NV], F32)
        k.vec_b = fw.buf("vec")
        k.ident_f = sbt(k, top, "ident_f", [128, 128], F32)
        k.ident_b = sbt(k, top, "ident_b", [128, 128], BF16)
        k.ones_b = sbt(k, top, "ones_b", [128, 128], BF16)
        k.ones_row = sbt(k, top, "ones_row", [1, 128], F32)
        k.cvec = sbt(k, top, "cvec", [128, 8], F32)
        k.eps_col = sbt(k, top, "eps_col", [128, 1], F32)
        k.const_b = fw.buf("const")
        cb = k.const_b
        fw.dma("sp", out=k.ident_f[:], in_=d["ident"], writes=[cb])
        fw.dma("pool", out=k.ident_b[:], in_=d["ident"], writes=[cb])
        fw.dma("sp", out=k.cvec[:], in_=d["cvec"], writes=[cb])
        fw.op("dve", lambda e: e.memset(k.ones_b[:], 1.0), writes=[cb])
        fw.op("dve", lambda e: e.memset(k.ones_row[:], 1.0), writes=[cb])
        fw.op("dve", lambda e: e.memset(k.eps_col[:], 1e-6), writes=[cb])
        fw.barrier()
        phase0(k)
        moe = (lambda l: phase_moe_sparse(k, l)) if sparse else (lambda l: phase_moe(k, l, n_exp, n_chunks))
        stages = [("mla", lambda: phase_mla(k)), ("moe0", lambda: moe(0)),
                  ("moba", lambda: phase_moba(k)), ("moe1", lambda: moe(1)),
                  ("final", lambda: phase_final(k))]
        for name, fn in stages:
            fn()
            if stop is not None and (stop == name or (name == "mla" and stop in ("1a", "1b")) or (name == "moba" and stop in ("3a", "3b"))):
                break
        if debug and stop not in ("1b", "3b"):
            for t in range(NT):
                ts_ = slice(t * 512, (t + 1) * 512)
                fw.dma("pool", out=k.dbg[:, :, ts_], in_=k.xT_d[:, :, ts_], reads=[k.xT_db[t]])
        fw.barrier()
        k.stats = dict(ninst=fw.ninst, nwaits=fw.nwaits, nsem=fw.nsem, counts={n_: e.sem.count for n_, e in fw.E.items()})
    return nc, k


def phase_moba(k):
    nc, fw, d = k.nc, k.fw, k.d
    kT_d = nc.dram_tensor("kT_scr", [8, 128, S], BF16, kind="Internal").ap()
    q_d = nc.dram_tensor("q_scr", [8, 128, S], BF16, kind="Internal").ap()
    V_d = nc.dram_tensor("V_scr", [8, 128, 32, 128], BF16, kind="Internal").ap()
    kd_b = [[fw.buf(f"kd{h}_{t}") for t in range(NT)] for h in range(8)]
    qd_b = [[fw.buf(f"qd{h}_{t}") for t in range(NT)] for h in range(8)]
    vd_b = [fw.buf(f"vd{t}") for t in range(NT)]
    ps, psb = k.ps, k.psb
    with contextlib.ExitStack() as st:
        kmacc = sbt(k, st, "kmacc", [128, 8, 16], F32)
        kmacc_b = fw.buf("kmacc")
        with contextlib.ExitStack() as st1:
            cos = sbt(k, st1, "cos128", [128, S], F32)
            sin = sbt(k, st1, "sin128", [128, S], F32)
            trig_b = fw.buf("trig128")
            with contextlib.ExitStack() as st2:
                build_trig(k, st2, cos, sin, trig_b, 128, 1, 3)
                fw.barrier()
            w_kv = sbt(k, st1, "w_kv", [128, 8, 2048], BF16)
            w_kR = sbt(k, st1, "w_kR", [128, 8, 8, 128], BF16)
            w_q = sbt(k, st1, "w_q", [128, 8, 1024], BF16)
            w_qR = sbt(k, st1, "w_qR", [128, 8, 8, 128], BF16)
            wb = fw.buf("w_moba")
            kv_ap = d["moba_w_kv"].rearrange("(kc p) f -> p kc f", p=128)
            q_ap = d["moba_w_q"][0].rearrange("(kc p) f -> p kc f", p=128)
            fw.dma("pool", out=w_kv[:], in_=kv_ap, writes=[wb])
            fw.dma("pool", out=w_q[:], in_=q_ap, writes=[wb])
            for h in range(8):
                c0 = h * 128
                fw.dma("pool", out=w_kR[:, :, h, 0:64], in_=kv_ap[:, :, c0 + 64:c0 + 128], writes=[wb])
                fw.dma("pool", out=w_kR[:, :, h, 64:128], in_=kv_ap[:, :, c0:c0 + 64], writes=[wb])
                fw.dma("pool", out=w_qR[:, :, h, 0:64], in_=q_ap[:, :, c0 + 64:c0 + 128], writes=[wb])
                fw.dma("pool", out=w_qR[:, :, h, 64:128], in_=q_ap[:, :, c0:c0 + 64], writes=[wb])
            xTt = sbt(k, st1, "xT3", [128, 8, 512], F32)
            xTt_b = fw.buf("xT3")
            hq = sbt(k, st1, "hq3", [128, 8, 512], BF16)
            hkv = sbt(k, st1, "hkv3", [128, 8, 512], BF16)
            hq_b, hkv_b = fw.buf("hq3"), fw.buf("hkv3")
            NR = norm_alloc(k, st1, 512)
            t1 = [sbt(k, st1, "t13", [128, 512], F32) for _ in range(2)]
            t2 = [sbt(k, st1, "t23", [128, 512], F32) for _ in range(2)]
            t1_b, t2_b = fw.bufs(2, "t13"), fw.bufs(2, "t23")
            kf = sbt(k, st1, "kf3", [128, 512], F32)
            kf_b = fw.buf("kf3")
            ob = [sbt(k, st1, "ob3", [128, 512], BF16) for _ in range(4)]
            ob_b = fw.bufs(4, "ob3")
            vb = [sbt(k, st1, "vb3", [128, 1024], BF16) for _ in range(2)]
            vb_b = fw.bufs(2, "vb3")
            ti, oi, vi = 0, 0, 0
            for t in range(NT):
                ts_ = slice(t * 512, (t + 1) * 512)
                fw.dma("sp", out=xTt[:], in_=k.xT_d[:, :, ts_], reads=[k.xT_db[t]], writes=[xTt_b])
                norm_rstd(k, NR, xTt, xTt_b, slice(0, 512), 0)
                norm_apply(k, NR, xTt, xTt_b, slice(0, 512), V_GSA[1], V_MOD[1] + 0, lambda kc: hq[:, kc, :], hq_b)
                norm_apply(k, NR, xTt, xTt_b, slice(0, 512), V_GSKV, V_KVMOD, lambda kc: hkv[:, kc, :], hkv_b)
                for h in range(8):
                    for which in range(2):
                        W, WR, hin, hin_b = (w_kv, w_kR, hkv, hkv_b) if which == 0 else (w_q, w_qR, hq, hq_b)
                        pa, pb_ = (1, 2) if which == 0 else (3, 4)
                        for kc in range(8):
                            fw.op("pe", lambda e: e.matmul(ps[pa][:], lhsT=W[:, kc, h * 128:(h + 1) * 128], rhs=hin[:, kc, :],
                                                           start=(kc == 0), stop=(kc == 7)),
                                  reads=[wb, hin_b], writes=[psb[pa]], inc=(kc == 7))
                        for kc in range(8):
                            fw.op("pe", lambda e: e.matmul(ps[pb_][:], lhsT=WR[:, kc, h, :], rhs=hin[:, kc, :],
                                                           start=(kc == 0), stop=(kc == 7)),
                                  reads=[wb, hin_b], writes=[psb[pb_]], inc=(kc == 7))
                        i = ti % 2
                        ti += 1
                        fw.op("dve", lambda e: e.tensor_tensor(out=t1[i][:], in0=ps[pa][:], in1=cos[:, ts_], op=ALU.mult),
                              reads=[psb[pa], trig_b], writes=[t1_b[i]])
                        fw.op("dve", lambda e: e.tensor_tensor(out=t2[i][:], in0=ps[pb_][:], in1=sin[:, ts_], op=ALU.mult),
                              reads=[psb[pb_], trig_b], writes=[t2_b[i]])
                        o = oi % 4
                        oi += 1
                        if which == 0:
                            fw.op("pool", lambda e: e.tensor_tensor(out=kf[:], in0=t1[i][:], in1=t2[i][:], op=ALU.add),
                                  reads=[t1_b[i], t2_b[i]], writes=[kf_b])
                            fw.op("act", lambda e: e.activation(out=ob[o][:], in_=kf[:], func=AF.Copy), reads=[kf_b], writes=[ob_b[o]])
                            fw.op("dve", lambda e: e.tensor_reduce(out=kmacc[:, h, 2 * t:2 * t + 2],
                                                                   in_=kf[:].rearrange("p (b c) -> p b c", b=2), axis=AX.X, op=ALU.add),
                                  reads=[kf_b], writes=[kmacc_b])
                            fw.dma("sp", out=kT_d[h, :, ts_], in_=ob[o][:], reads=[ob_b[o]], writes=[kd_b[h][t]], sem_owner=ob_b[o])
                        else:
                            fw.op("pool", lambda e: e.tensor_tensor(out=ob[o][:], in0=t1[i][:], in1=t2[i][:], op=ALU.add),
                                  reads=[t1_b[i], t2_b[i]], writes=[ob_b[o]])
                            fw.dma("sp", out=q_d[h, :, ts_], in_=ob[o][:], reads=[ob_b[o]], writes=[qd_b[h][t]], sem_owner=ob_b[o])
                for sub in range(4):
                    tk = slice(sub * 128, (sub + 1) * 128)
                    v = vi % 2
                    vi += 1
                    for half in range(2):
                        pv = 5 + half
                        for kc in range(8):
                            fw.op("pe", lambda e: e.matmul(ps[pv][:], lhsT=hkv[:, kc, tk], rhs=w_kv[:, kc, 1024 + half * 512:1024 + (half + 1) * 512],
                                                           start=(kc == 0), stop=(kc == 7)),
                                  reads=[wb, hkv_b], writes=[psb[pv]], inc=(kc == 7))
                        fw.op("act", lambda e: e.activation(out=vb[v][:, half * 512:(half + 1) * 512], in_=ps[pv][:], func=AF.Copy),
                              reads=[psb[pv]], writes=[vb_b[v]])
                    stile = t * 4 + sub
                    fw.dma("sp", out=V_d[:, :, stile, :].rearrange("h p d -> p h d"), in_=vb[v][:].rearrange("p (h d) -> p h d", h=8),
                           reads=[vb_b[v]], writes=[vd_b[t]], sem_owner=vb_b[v])
            fw.barrier(release=[wb, xTt_b] + ob_b + vb_b)
        if k.stop == "3a":
            return
        OT = sbt(k, st, "OT2", [128, 8, S], BF16)
        OT_b = [[fw.buf(f"OTb{h}_{t}") for t in range(NT)] for h in range(8)]
        with contextlib.ExitStack() as st1:
            kT = [sbt(k, st1, "kT3", [128, S], BF16) for _ in range(2)]
            qT = [sbt(k, st1, "qT3", [128, S], BF16) for _ in range(2)]
            Vh = [sbt(k, st1, "Vh3", [128, 32, 128], BF16) for _ in range(2)]
            kT_b, qT_b, Vh_b = fw.bufs(2, "kT3"), fw.bufs(2, "qT3"), fw.bufs(2, "Vh3")
            mbT = sbt(k, st1, "mbT", [16, S], BF16)
            mbT_b = [fw.buf(f"mbT{i}") for i in range(32)]
            oh = sbt(k, st1, "oh16", [16, 16, 128], BF16)
            cm = sbt(k, st1, "cm256", [128, 2, 256], BF16)
            pastm = sbt(k, st1, "pastm", [128, 16, 16], F32)
            val01 = sbt(k, st1, "val01", [128, 16, 16], F32)
            cst_b = fw.buf("cst3")
            fw.dma("pool", out=oh[:], in_=d["oh16"], writes=[cst_b])
            fw.dma("pool", out=cm[:], in_=d["cm256"], writes=[cst_b])
            fw.dma("sp", out=pastm[:], in_=d["pastmask"], writes=[cst_b])
            fw.dma("sp", out=val01[:], in_=d["valid01"], writes=[cst_b])
            kmb = sbt(k, st1, "kmb", [128, 8, 16], BF16)
            kmb_b = fw.buf("kmb")
            fw.op("dve", lambda e: e.tensor_scalar(out=kmb[:], in0=kmacc[:], scalar1=1.0 / 256, scalar2=None, op0=ALU.mult),
                  reads=[kmacc_b], writes=[kmb_b])
            gm = sbt(k, st1, "gm", [128, 16], F32)
            mx = sbt(k, st1, "gmx", [128, 8], F32)
            m1 = sbt(k, st1, "gm1", [128, 16], F32)
            mbs = sbt(k, st1, "gmbs", [128, 16], BF16)
            g_b = fw.buf("gate3")
            PT = [sbt(k, st1, "PT3", [128, 256], BF16) for _ in range(3)]
            PT_b = fw.bufs(3, "PT3")
            rden = sbt(k, st1, "rden3", [128, 256], F32)
            rden_b = fw.buf("rden3")
            scale = float(128 ** -0.5)
            si = 0

            def load_head(h):
                j = h % 2
                fw.dma("sp", out=kT[j][:], in_=kT_d[h], reads=[b for b in kd_b[h]], writes=[kT_b[j]])
                fw.dma("sp", out=qT[j][:], in_=q_d[h], reads=[b for b in qd_b[h]], writes=[qT_b[j]])
                fw.dma("sp", out=Vh[j][:], in_=V_d[h], reads=vd_b, writes=[Vh_b[j]])
            load_head(0)
            for h in range(8):
                j = h % 2
                if h + 1 < 8:
                    load_head(h + 1)
                for qt in range(32):
                    own = qt // 2
                    qsl = slice(qt * 128, (qt + 1) * 128)
                    fw.op("pe", lambda e: e.matmul(ps[7][:, 0:16], lhsT=qT[j][:, qsl], rhs=kmb[:, h, :], start=True, stop=True),
                          reads=[qT_b[j], kmb_b], writes=[psb[7]])
                    fw.op("dve", lambda e: e.tensor_tensor(out=gm[:], in0=ps[7][:, 0:16], in1=pastm[:, own, :], op=ALU.add),
                          reads=[psb[7], cst_b], writes=[g_b])
                    fw.op("dve", lambda e: e.max(out=mx[:], in_=gm[:]), reads=[g_b], writes=[g_b])
                    fw.op("dve", lambda e: e.scalar_tensor_tensor(out=m1[:], in0=gm[:], scalar=mx[:, 2:3], in1=val01[:, own, :],
                                                                  op0=ALU.is_ge, op1=ALU.mult), reads=[g_b, cst_b], writes=[g_b])
                    fw.op("dve", lambda e: e.tensor_scalar(out=mbs[:], in0=m1[:], scalar1=-1.0, scalar2=-NEG, op0=ALU.add, op1=ALU.mult),
                          reads=[g_b], writes=[g_b])
                    fw.op("pe", lambda e: e.matmul(ps[6][0:16, 0:128], lhsT=mbs[:], rhs=k.ident_b[:], start=True, stop=True),
                          reads=[g_b, k.const_b], writes=[psb[6]])
                    fw.op("act", lambda e: e.activation(out=mbT[:, qsl], in_=ps[6][0:16, 0:128], func=AF.Copy),
                          reads=[psb[6]], writes=[mbT_b[qt]])
                tiles = [(B, kt) for B in range(16) for kt in range(2 * B + 2)]

                def emit_S(B, kt, pi):
                    qs = slice(B * 256, (B + 1) * 256)
                    ks = slice(kt * 128, (kt + 1) * 128)
                    n = kt // 2
                    fw.op("pe", lambda e: e.matmul(ps[pi][:, 0:256], lhsT=kT[j][:, ks], rhs=qT[j][:, qs], start=True, stop=False),
                          reads=[kT_b[j], qT_b[j]], writes=[psb[pi]], inc=False)
                    if n < B:
                        fw.op("pe", lambda e: e.matmul(ps[pi][:, 0:256], lhsT=oh[:, n, :], rhs=mbT[:, qs], start=False, stop=True),
                              reads=[cst_b, mbT_b[2 * B], mbT_b[2 * B + 1]], writes=[psb[pi]])
                    else:
                        fw.op("pe", lambda e: e.matmul(ps[pi][:, 0:256], lhsT=k.ident_b[:], rhs=cm[:, kt - 2 * B, :], start=False, stop=True),
                              reads=[cst_b, k.const_b], writes=[psb[pi]])

                emit_S(tiles[0][0], tiles[0][1], si % 3)
                for ti_, (B, kt) in enumerate(tiles):
                    qs = slice(B * 256, (B + 1) * 256)
                    po, pd = 3 + (B % 2), 5 + (B % 2)
                    nk = 2 * B + 2
                    pi = si % 3
                    si += 1
                    fw.op("act", lambda e: e.activation(out=PT[pi][:], in_=ps[pi][:, 0:256], func=AF.Exp, scale=scale),
                          reads=[psb[pi]], writes=[PT_b[pi]])
                    if ti_ + 1 < len(tiles):
                        emit_S(tiles[ti_ + 1][0], tiles[ti_ + 1][1], si % 3)
                    fw.op("pe", lambda e: e.matmul(ps[po][:, 0:256], lhsT=Vh[j][:, kt, :], rhs=PT[pi][:], start=(kt == 0), stop=(kt == nk - 1)),
                          reads=[Vh_b[j], PT_b[pi]], writes=[psb[po]], inc=False)
                    fw.op("pe", lambda e: e.matmul(ps[pd][:, 0:256], lhsT=k.ones_b[:], rhs=PT[pi][:], start=(kt == 0), stop=(kt == nk - 1)),
                          reads=[k.const_b, PT_b[pi]], writes=[psb[pd]], inc=True)
                    if kt == nk - 1:
                        fw.op("dve", lambda e: e.reciprocal(out=rden[:], in_=ps[pd][:, 0:256]), reads=[psb[pd]], writes=[rden_b])
                        fw.op("dve", lambda e: e.tensor_tensor(out=OT[:, h, qs], in0=ps[po][:, 0:256], in1=rden[:], op=ALU.mult),
                              reads=[psb[po], rden_b], writes=[OT_b[h][B // 2]])
            fw.barrier(release=kT_b + qT_b + Vh_b + [cst_b])
        if k.stop == "3b":
            dump_OT(k, OT, OT_b)
            return
        out_proj(k, OT, OT_b, d["moba_w_o"][0], V_MOD[1] + 16)


def make_in_maps(inputs):
    consts = make_consts()
    x = np.asarray(inputs["x"], dtype=np.float32)
    c = np.asarray(inputs["c"], dtype=np.float32)
    pos = np.asarray(inputs["positions"]).astype(np.int32)
    shared = {}
    for n_ in INPUT_NAMES:
        shared[n_] = np.ascontiguousarray(np.asarray(inputs[n_], dtype=np.float32).reshape(INPUT_SHAPES[n_]))
    shared.update(consts)
    in_maps = []
    for b in range(8):
        m = dict(shared)
        m["x"] = np.ascontiguousarray(x[b])
        m["cT"] = np.ascontiguousarray(c[b].reshape(8, 128).T)
        m["pos"] = np.ascontiguousarray(pos[b][None, :])
        in_maps.append(m)
    return in_maps


_CACHE = {}


def kernel(**inputs):
    if "nc" not in _CACHE:
        _CACHE["nc"] = build_program()[0]
    nc = _CACHE["nc"]
    in_maps = make_in_maps(inputs)
    res = run_bass_kernel_spmd(nc, in_maps, core_ids=list(range(8)))
    return np.stack([np.asarray(r["out"]) for r in res.results], axis=0).astype(np.float32)
```
